# Optimizing a Trainium2 kernel written in Bass

```python
import jax, jax.numpy as jnp
from jax import lax
import numpy as np

D_MODEL = 1024
BATCH = 16
SEQ = 2048
DEPTH = 1

GRID_W = 64
CTX_LEN = 256
MIX_WIDTH = D_MODEL
POOL_WIDTH = MIX_WIDTH // 2
POOL_WINDOWS = (2, 4, 8, 16)
POOL_GROUP = POOL_WIDTH // len(POOL_WINDOWS)
MLA_HEADS = 8
QK_NOPE = 64
QK_ROPE = 32
QK_HEAD = QK_NOPE + QK_ROPE
V_HEAD = 64
Q_LORA = 384
KV_LORA = 256
ROPE_THETA = 10000.0
ATTN_BLOCK = 128
IN_WIDTH = POOL_WIDTH + Q_LORA + KV_LORA + QK_ROPE
PEER_HEADS = 8
PEER_N_KEYS = 128
PEER_N_EXPERTS = PEER_N_KEYS * PEER_N_KEYS
PEER_QUERY_DIM = 256
PEER_HALF = PEER_QUERY_DIM // 2
PEER_TOPK = 16
PEER_BLOCK = 128
EPS = 1e-6

kernel_name = "hymba_pool_mla_peer_dit_layer"


def rms_norm(x, g):
    xf = x.astype(jnp.float32)
    y = xf * lax.rsqrt(jnp.mean(xf * xf, axis=-1, keepdims=True) + EPS)
    return (y * g.astype(jnp.float32)).astype(x.dtype)


def modulate(h, shift, scale):
    return h * (1.0 + scale[..., None, :]) + shift[..., None, :]


def axial_rope_tables(n_tokens):
    rows = n_tokens // GRID_W
    row = jnp.repeat(jnp.arange(rows), GRID_W).astype(jnp.float32)
    col = jnp.tile(jnp.arange(GRID_W), rows).astype(jnp.float32)
    n = QK_ROPE // 4
    inv = 1.0 / (ROPE_THETA ** (jnp.arange(n, dtype=jnp.float32) / n))
    ang_r = row[:, None] * inv
    ang_c = col[:, None] * inv
    ang = jnp.concatenate([ang_r, ang_r, ang_c, ang_c], axis=-1)
    return jnp.cos(ang), jnp.sin(ang)


def apply_rope(t, cos, sin):
    nope, rope = t[..., :QK_NOPE], t[..., QK_NOPE:]
    n = QK_ROPE // 4
    r = rope.reshape(rope.shape[:-1] + (2, 2, n))
    rot = jnp.stack([-r[..., 1, :], r[..., 0, :]], axis=-2).reshape(rope.shape)
    rope = rope * cos[:, None, :] + rot * sin[:, None, :]
    return jnp.concatenate([nope, rope.astype(t.dtype)], axis=-1)


def multiscale_pool(p, pool_w, pool_scale):
    B, L, C = p.shape
    pf = p.astype(jnp.float32)
    cs = jnp.concatenate([jnp.zeros((B, 1, C), jnp.float32), jnp.cumsum(pf, axis=1)], axis=1)
    t = jnp.arange(L)
    outs = []
    for g, w in enumerate(POOL_WINDOWS):
        lo = jnp.clip(t - w // 2, 0, L)
        hi = jnp.clip(t + w // 2, 0, L)
        sl = slice(g * POOL_GROUP, (g + 1) * POOL_GROUP)
        csg = cs[..., sl]
        mean = (jnp.take(csg, hi, axis=1) - jnp.take(csg, lo, axis=1)) / (hi - lo).astype(jnp.float32)[:, None]
        outs.append(jnp.einsum('blc,cd->bld', mean - pf[..., sl], pool_w[g].astype(jnp.float32)))
    y = jnp.concatenate(outs, axis=-1) * pool_scale.astype(jnp.float32)
    return y.astype(p.dtype)


def mla_keys_values(ckv, kr, g_kv_lora, w_kv_up, g_qk_k):
    B, L, _ = ckv.shape
    kv = (rms_norm(ckv, g_kv_lora) @ w_kv_up).reshape(B, L, MLA_HEADS, QK_NOPE + V_HEAD)
    k_nope, v = kv[..., :QK_NOPE], kv[..., QK_NOPE:]
    k = jnp.concatenate([k_nope, jnp.broadcast_to(kr[:, :, None, :], (B, L, MLA_HEADS, QK_ROPE))], axis=-1)
    return rms_norm(k, g_qk_k), v


def block_attention(q, k_all, v_all):
    B, S, H, _ = q.shape
    nb = S // ATTN_BLOCK
    qb = q.reshape(B, nb, ATTN_BLOCK, H, QK_HEAD).transpose(1, 0, 3, 2, 4)
    kt = k_all.transpose(0, 2, 1, 3)
    vt = v_all.transpose(0, 2, 1, 3)
    scale = 1.0 / np.sqrt(QK_HEAD)

    def one_block(qblk):
        s = jnp.einsum('bhqd,bhkd->bhqk', qblk, kt).astype(jnp.float32) * scale
        pr = jax.nn.softmax(s, axis=-1)
        return jnp.einsum('bhqk,bhkd->bhqd', pr.astype(vt.dtype), vt)

    o = lax.map(one_block, qb)
    return o.transpose(1, 0, 3, 2, 4).reshape(B, S, H * V_HEAD)


def peer_ffn(h, peer_w_q, peer_sub_keys, peer_u, peer_v):
    B, L, D = h.shape
    qp = (h @ peer_w_q).reshape(B, L, PEER_HEADS, 2, PEER_HALF)
    s_half = jnp.einsum('blhpd,pnd->blhpn', qp, peer_sub_keys).astype(jnp.float32)
    v1, i1 = lax.top_k(s_half[..., 0, :], PEER_TOPK)
    v2, i2 = lax.top_k(s_half[..., 1, :], PEER_TOPK)
    cand = (v1[..., :, None] + v2[..., None, :]).reshape(B, L, PEER_HEADS, PEER_TOPK * PEER_TOPK)
    cidx = (i1[..., :, None] * PEER_N_KEYS + i2[..., None, :]).reshape(B, L, PEER_HEADS, PEER_TOPK * PEER_TOPK)
    top, pos = lax.top_k(cand, PEER_TOPK)
    eidx = jnp.take_along_axis(cidx, pos, axis=-1)
    gates = jax.nn.softmax(top, axis=-1)
    T = B * L
    nblk = T // PEER_BLOCK
    xt = h.reshape(nblk, PEER_BLOCK, D)
    et = eidx.reshape(nblk, PEER_BLOCK, PEER_HEADS * PEER_TOPK)
    gt = gates.reshape(nblk, PEER_BLOCK, PEER_HEADS * PEER_TOPK)

    def expert_block(args):
        xb, eb, gb = args
        u = jnp.take(peer_u, eb, axis=0)
        vv = jnp.take(peer_v, eb, axis=0)
        a = jax.nn.gelu(jnp.einsum('td,ted->te', xb, u).astype(jnp.float32), approximate=False)
        return jnp.einsum('te,ted->td', (gb * a).astype(xb.dtype), vv)

    y = lax.map(expert_block, (xt, et, gt))
    return y.reshape(B, L, D)


def setup_inputs(seed: int = 0) -> dict:
    key = jax.random.key(seed)
    ks = jax.random.split(key, 22)
    f = jnp.float32
    nrm = lambda k, shape, s: jax.random.normal(k, shape, f) * s
    return {
        "x": nrm(ks[0], (BATCH, SEQ, D_MODEL), 1.0),
        "c": nrm(ks[1], (BATCH, D_MODEL), 1.0),
        "ctx": nrm(ks[2], (BATCH, CTX_LEN, D_MODEL), 1.0),
        "c_ctx": nrm(ks[3], (D_MODEL,), 1.0),
        "w_ada": nrm(ks[4], (D_MODEL, 6 * D_MODEL), 0.5 * D_MODEL ** -0.5),
        "b_ada": nrm(ks[5], (6 * D_MODEL,), 0.02),
        "g_norm1": 1.0 + nrm(ks[6], (D_MODEL,), 0.02),
        "w_in": nrm(ks[7], (D_MODEL, IN_WIDTH), D_MODEL ** -0.5),
        "pool_w": nrm(ks[8], (len(POOL_WINDOWS), POOL_GROUP, POOL_GROUP), POOL_GROUP ** -0.5),
        "pool_scale": 1.0 + nrm(ks[9], (POOL_WIDTH,), 0.1),
        "g_q_lora": 1.0 + nrm(ks[10], (Q_LORA,), 0.02),
        "w_q_up": nrm(ks[11], (Q_LORA, MLA_HEADS * QK_HEAD), Q_LORA ** -0.5),
        "g_kv_lora": 1.0 + nrm(ks[12], (KV_LORA,), 0.02),
        "w_kv_up": nrm(ks[13], (KV_LORA, MLA_HEADS * (QK_NOPE + V_HEAD)), KV_LORA ** -0.5),
        "g_qk_q": 1.0 + nrm(ks[14], (QK_HEAD,), 0.02),
        "g_qk_k": 1.0 + nrm(ks[15], (QK_HEAD,), 0.02),
        "w_out": nrm(ks[16], (MIX_WIDTH, D_MODEL), MIX_WIDTH ** -0.5),
        "g_norm2": 1.0 + nrm(ks[17], (D_MODEL,), 0.02),
        "peer_w_q": nrm(ks[18], (D_MODEL, PEER_HEADS * PEER_QUERY_DIM), D_MODEL ** -0.5),
        "peer_sub_keys": nrm(ks[19], (2, PEER_N_KEYS, PEER_HALF), PEER_HALF ** -0.5),
        "peer_u": nrm(ks[20], (PEER_N_EXPERTS, D_MODEL), D_MODEL ** -0.5),
        "peer_v": nrm(ks[21], (PEER_N_EXPERTS, D_MODEL), 0.5),
    }


def hybrid_layer(x, c, ctx, c_ctx, w_ada, b_ada, g_norm1, w_in, pool_w, pool_scale,
                 g_q_lora, w_q_up, g_kv_lora, w_kv_up, g_qk_q, g_qk_k, w_out,
                 g_norm2, peer_w_q, peer_sub_keys, peer_u, peer_v):
    B, S, D = x.shape
    mod = jax.nn.silu(c) @ w_ada + b_ada
    shift1, scale1, gate1, shift2, scale2, gate2 = jnp.split(mod, 6, axis=-1)
    mod_ctx = jax.nn.silu(c_ctx) @ w_ada[:, :2 * D] + b_ada[:2 * D]
    shift1_c, scale1_c = mod_ctx[:D], mod_ctx[D:]

    h1 = modulate(rms_norm(x, g_norm1), shift1, scale1)
    proj = h1 @ w_in
    o1 = POOL_WIDTH
    o2 = o1 + Q_LORA
    o3 = o2 + KV_LORA
    p_in, cq, ckv, kr = proj[..., :o1], proj[..., o1:o2], proj[..., o2:o3], proj[..., o3:]

    pool_out = multiscale_pool(p_in, pool_w, pool_scale)

    cos, sin = axial_rope_tables(S)
    q = (rms_norm(cq, g_q_lora) @ w_q_up).reshape(B, S, MLA_HEADS, QK_HEAD)
    q = apply_rope(rms_norm(q, g_qk_q), cos, sin)
    k_lat, v_lat = mla_keys_values(ckv, kr, g_kv_lora, w_kv_up, g_qk_k)
    k_lat = apply_rope(k_lat, cos, sin)
    hc = modulate(rms_norm(ctx, g_norm1), shift1_c, scale1_c)
    projc = hc @ w_in[:, o2:]
    k_ctx, v_ctx = mla_keys_values(projc[..., :KV_LORA], projc[..., KV_LORA:], g_kv_lora, w_kv_up, g_qk_k)
    k_all = jnp.concatenate([k_lat, k_ctx], axis=1)
    v_all = jnp.concatenate([v_lat, v_ctx], axis=1)
    mla_out = block_attention(q, k_all, v_all)

    mix = jnp.concatenate([pool_out, mla_out], axis=-1) @ w_out
    x = x + gate1[:, None, :] * mix

    h2 = modulate(rms_norm(x, g_norm2), shift2, scale2)
    x = x + gate2[:, None, :] * peer_ffn(h2, peer_w_q, peer_sub_keys, peer_u, peer_v)
    return x


def reference(x, c, ctx, c_ctx, w_ada, b_ada, g_norm1, w_in, pool_w, pool_scale,
              g_q_lora, w_q_up, g_kv_lora, w_kv_up, g_qk_q, g_qk_k, w_out,
              g_norm2, peer_w_q, peer_sub_keys, peer_u, peer_v):
    for _ in range(DEPTH):
        x = hybrid_layer(x, c, ctx, c_ctx, w_ada, b_ada, g_norm1, w_in, pool_w, pool_scale,
                         g_q_lora, w_q_up, g_kv_lora, w_kv_up, g_qk_q, g_qk_k, w_out,
                         g_norm2, peer_w_q, peer_sub_keys, peer_u, peer_v)
    return x
```

```python
import numpy as np
import ml_dtypes
import concourse.bass as bass
import concourse.mybir as mybir
from concourse.bass_utils import run_bass_kernel_spmd

F32 = mybir.dt.float32
BF16 = mybir.dt.bfloat16
U32 = mybir.dt.uint32
I32 = mybir.dt.int32
AF = mybir.ActivationFunctionType
ALU = mybir.AluOpType
AX = mybir.AxisListType

NCORES = 8
NB = 2
S_ = 2048
CTX = 256
D = 1024
DC = 8
KT = (S_ + CTX) // 128
EPS = 1e-6
ST = 256
TN = 256
NST = NB * S_ // ST


class Res:
    __slots__ = ("name", "last_w", "readers")

    def __init__(self, name):
        self.name = name
        self.last_w = None
        self.readers = {}


class Sched:
    ENG = ("pe", "act", "dve", "pool", "sp")

    def __init__(self, nc, n_dma_sems=32):
        self.nc = nc
        self.sem = {e: nc.alloc_semaphore("prog_" + e) for e in self.ENG}
        self.cnt = {e: 0 for e in self.ENG}
        self.dsem = {"sp": [nc.alloc_semaphore("dma_%d" % i) for i in range(n_dma_sems)],
                     "pool": [nc.alloc_semaphore("dmap_%d" % i) for i in range(8)]}
        self.dcnt = {q: [0] * len(v) for q, v in self.dsem.items()}
        self.dnext = {q: 0 for q in self.dsem}
        self.seen = {e: {} for e in self.ENG}
        self.prog = {e: [] for e in self.ENG}

    def _wait(self, eng, ev):
        if ev is None:
            return
        s, v = ev
        k = id(s)
        if self.seen[eng].get(k, 0) >= v:
            return
        self.seen[eng][k] = v
        self.prog[eng].append(("w", s, v))

    def _deps(self, eng, reads, writes, own_sem):
        skip_own = own_sem if eng == "pe" else None
        for r in reads:
            self._wait(eng, r.last_w)
        for w in writes:
            if w.last_w is not None and w.last_w[0] is not skip_own:
                self._wait(eng, w.last_w)
            for ev in w.readers.values():
                if ev[0] is not skip_own:
                    self._wait(eng, ev)

    def _commit(self, ev, reads, writes):
        for w in writes:
            w.last_w = ev
            w.readers = {}
        for r in reads:
            if r not in writes:
                r.readers[id(ev[0])] = ev

    def op(self, eng, fn, reads=(), writes=()):
        own = self.sem[eng]
        self._deps(eng, reads, writes, own)
        self.cnt[eng] += 1
        ev = (own, self.cnt[eng])
        self.prog[eng].append(("o", fn, own, 1))
        self._commit(ev, reads, writes)
        return ev

    def dma(self, fn, reads=(), writes=(), q="sp", after=()):
        for ev in after:
            self._wait(q, ev)
        i = self.dnext[q]
        self.dnext[q] = (i + 1) % len(self.dsem[q])
        s = self.dsem[q][i]
        self._deps(q, reads, writes, None)
        if self.dcnt[q][i] > 0:
            self._wait(q, (s, 16 * self.dcnt[q][i]))
        self.dcnt[q][i] += 1
        ev = (s, 16 * self.dcnt[q][i])
        self.prog[q].append(("o", fn, s, 16))
        self._commit(ev, reads, writes)
        return ev

    def barrier(self):
        evs = [(self.sem[e], self.cnt[e]) for e in self.ENG if self.cnt[e] > 0]
        for q in self.dsem:
            evs += [(self.dsem[q][i], 16 * self.dcnt[q][i]) for i in range(len(self.dsem[q])) if self.dcnt[q][i] > 0]
        for e in self.ENG:
            for ev in evs:
                if ev[0] is not self.sem[e]:
                    self._wait(e, ev)

    def emit(self):
        nc = self.nc
        engmap = {"pe": "tensor", "act": "scalar", "dve": "vector", "pool": "gpsimd", "sp": "sync"}
        with nc.Block() as block:
            for e in self.ENG:
                prog = self.prog[e]

                def body(engine, prog=prog):
                    for it in prog:
                        if it[0] == "w":
                            engine.wait_ge(it[1], it[2])
                        else:
                            it[1](engine).then_inc(it[2], it[3])

                getattr(block, engmap[e])(body)


class SbufStack:
    def __init__(self, nc, lo=16640, hi=229344):
        self.nc = nc
        self.top = lo
        self.hi = hi
        self.n = 0
        self.cache = {}

    def alloc(self, shape, dtype, name=None):
        esz = {F32: 4, BF16: 2, U32: 4, I32: 4}[dtype]
        nbytes = int(np.prod(shape[1:])) * esz
        off = (self.top + 63) // 64 * 64
        assert off + nbytes <= self.hi, ("SBUF overflow", name, off, nbytes)
        self.top = off + nbytes
        self.n += 1
        key = (name, off, tuple(shape), str(dtype))
        if name is not None and key in self.cache:
            return self.cache[key]
        t = self.nc.alloc_sbuf_tensor_at(name or ("t%d" % self.n), list(shape), dtype, offset=off)
        self.cache[key] = t
        return t

    def mark(self):
        return self.top

    def release(self, m):
        self.top = m


def build_program(stage="full"):
    nc = bass.Bass("TRN2", target_bir_lowering=False)

    def din(name, shape, dt=F32):
        return nc.dram_tensor(name, list(shape), dt, kind="ExternalInput").ap()

    dbg = stage != "full"
    xT_d = din("xT", [NB, 128, DC, S_])
    x_d = din("x", [NB, S_, D])
    ctxT_d = din("ctxT", [NB, 128, DC, CTX])
    cT_d = din("cT", [128, DC, 3])
    w_ada_d = din("w_ada", [D, 6 * D])
    b_ada_d = din("b_ada", [6 * D])
    g1_d = din("g_norm1", [D])
    w_in_d = din("w_in", [D, 1184])
    pool_w_d = din("pool_w", [4, 128, 128])
    pool_scale_d = din("pool_scale", [512])
    gq_d = din("g_q_lora", [384])
    w_q_up_d = din("w_q_up", [384, 768])
    gkv_d = din("g_kv_lora", [256])
    w_kv_up_d = din("w_kv_up", [256, 1024])
    gqkq_d = din("g_qk_q", [96])
    gqkk_d = din("g_qk_k", [96])
    w_out_d = din("w_out", [D, D])
    g2_d = din("g_norm2", [D])
    wqT_d = din("wqT", [2048, D])
    keysT_d = din("keysT", [2, 128, 128])
    uT_d = din("uT", [128, 128, 1024])
    v_d = din("pv", [128, 128, 1024])
    ident_d = din("ident", [128, 128])
    rope_d = din("ropecs", [128, 16, 2, 32])
    invcnt_d = din("invcnt", [4, 16])
    iota_d = din("iota", [128])

    out_kind = "ExternalOutput"
    out_d = nc.dram_tensor("out", [NB, S_, D], F32, kind=out_kind).ap()
    skind = "ExternalOutput" if dbg else "Internal"
    x1_s = nc.dram_tensor("x1_s", [NB * S_, D], F32, kind=skind).ap()
    h2T_s = nc.dram_tensor("h2T_s", [NST, 128, DC, ST], BF16, kind=skind).ap()
    ub_s = nc.dram_tensor("ub_s", [128, 128, 1024], BF16, kind="Internal").ap()
    vb_s = nc.dram_tensor("vb_s", [128, 128, 1024], BF16, kind="Internal").ap()
    wp_s = nc.dram_tensor("wp_s", [128, DC, 2048], BF16, kind="Internal").ap()

    S = Sched(nc)
    sb = SbufStack(nc)

    PS = [nc.alloc_psum_tensor("psb%d" % i, [128, 512], F32) for i in range(8)]
    PSR = [Res("psb%d" % i) for i in range(8)]

    def V(fn, r=(), w=()):
        return S.op("dve", fn, r, w)

    def A(fn, r=(), w=()):
        return S.op("act", fn, r, w)

    def G(fn, r=(), w=()):
        return S.op("pool", fn, r, w)

    def T(fn, r=(), w=()):
        return S.op("pe", fn, r, w)

    def DMA(fn, r=(), w=()):
        return S.dma(fn, r, w)

    ident_f = sb.alloc([128, 128], F32, "ident_f")
    ident_b = sb.alloc([128, 128], BF16, "ident_b")
    ones_f = sb.alloc([128, 128], F32, "ones_f")
    ones_b = sb.alloc([128, 128], BF16, "ones_b")
    iota_f = sb.alloc([128, 128], F32, "iota_f")
    iota_b = sb.alloc([128, 128], BF16, "iota_b")
    eps_t = sb.alloc([128, 1], F32, "eps_t")
    R_const = Res("const")
    DMA(lambda e: e.dma_start(out=ident_f[:], in_=ident_d[:, :]), w=[R_const])
    DMA(lambda e: e.dma_start(out=iota_f[:], in_=iota_d.partition_broadcast(128)), w=[R_const])
    V(lambda e: e.tensor_copy(out=ident_b[:], in_=ident_f[:]), r=[R_const], w=[R_const])
    V(lambda e: e.tensor_copy(out=iota_b[:], in_=iota_f[:]), r=[R_const], w=[R_const])
    V(lambda e: e.memset(ones_f[:], 1.0), w=[R_const])
    V(lambda e: e.memset(ones_b[:], 1.0), w=[R_const])
    V(lambda e: e.memset(eps_t[:], EPS), w=[R_const])

    modfm = sb.alloc([128, 16, 3], F32, "modfm")
    gs1 = sb.alloc([128, DC, 3], F32, "gs1")
    silT = sb.alloc([128, DC, 3], F32, "silT")
    gate2_bc = sb.alloc([128, NB, D], F32, "gate2_bc")
    gq_fm = sb.alloc([128, 3], F32, "gq_fm")
    gkv_fm = sb.alloc([128, 2], F32, "gkv_fm")
    psc_fm = sb.alloc([128, 4], F32, "psc_fm")
    R_mod = Res("modfm")
    R_g2bc = [Res("gate2bc%d" % b) for b in range(NB)]
    persist_mark = sb.mark()

    m0 = sb.mark()
    cT = sb.alloc([128, DC, 3], F32, "cT")
    g1_fm = sb.alloc([128, DC], F32, "g1_fm")
    bfm = sb.alloc([128, 16], F32, "bfm")
    wada_c = [sb.alloc([128, DC, 128], F32, "wada_c%d" % i) for i in range(2)]
    R_wc = [Res("wadac%d" % i) for i in range(2)]
    R_t0 = Res("p0tmp")
    DMA(lambda e: e.dma_start(out=cT[:], in_=cT_d[:, :, :]), w=[R_t0])
    DMA(lambda e: e.dma_start(out=g1_fm[:], in_=g1_d.rearrange("(c p) -> p c", p=128), allow_slow_non_contiguous=True), w=[R_t0])
    DMA(lambda e: e.dma_start(out=bfm[:], in_=b_ada_d[0:2048].rearrange("(c p) -> p c", p=128), allow_slow_non_contiguous=True), w=[R_t0])
    DMA(lambda e: e.dma_start(out=gq_fm[:], in_=gq_d.rearrange("(c p) -> p c", p=128), allow_slow_non_contiguous=True), w=[R_mod])
    DMA(lambda e: e.dma_start(out=gkv_fm[:], in_=gkv_d.rearrange("(c p) -> p c", p=128), allow_slow_non_contiguous=True), w=[R_mod])
    DMA(lambda e: e.dma_start(out=psc_fm[:], in_=pool_scale_d.rearrange("(c p) -> p c", p=128), allow_slow_non_contiguous=True), w=[R_mod])
    A(lambda e: e.activation(out=silT[:], in_=cT[:], func=AF.Silu), r=[R_t0], w=[R_mod])
    for j in range(16):
        wb = wada_c[j % 2]
        DMA(lambda e, wb=wb, j=j: e.dma_start(
            out=wb[:], in_=w_ada_d[:, j * 128:(j + 1) * 128].rearrange("(c p) n -> p c n", p=128)),
            w=[R_wc[j % 2]])
        for dc in range(DC):
            T(lambda e, wb=wb, j=j, dc=dc: e.matmul(
                PS[0][:, j * 4:j * 4 + 3], lhsT=wb[:, dc, :], rhs=silT[:, dc, :],
                start=(dc == 0), stop=(dc == DC - 1)),
                r=[R_wc[j % 2], R_mod], w=[PSR[0]])
    V(lambda e: e.tensor_tensor(
        out=modfm[:], in0=PS[0][:, 0:64].rearrange("p (j c) -> p j c", c=4)[:, :, 0:3],
        in1=bfm[:].unsqueeze(2).to_broadcast([128, 16, 3]), op=ALU.add),
        r=[PSR[0], R_t0], w=[R_mod])
    for col in range(3):
        V(lambda e, col=col: e.scalar_tensor_tensor(
            out=gs1[:, :, col], in0=modfm[:, 8:16, col], scalar=1.0, in1=g1_fm[:],
            op0=ALU.add, op1=ALU.mult), r=[R_mod, R_t0], w=[R_mod])
    S.barrier()
    sb.release(m0)

    R_ub = [Res("ub%d" % g) for g in range(32)]
    R_vb = [Res("vb%d" % g) for g in range(32)]
    cast_jobs = []
    if stage in ("full", "peer"):
        for g in range(32):
            for (src_t, dst_t, RR) in ((uT_d, ub_s, R_ub), (v_d, vb_s, R_vb)):
                cast_jobs.append((src_t, dst_t, RR, g))

    def cast_some(n, gate):
        for _ in range(n):
            if not cast_jobs:
                return
            src_t, dst_t, RR, g = cast_jobs.pop(0)
            S.dma(lambda e, src_t=src_t, dst_t=dst_t, g=g: e.dma_start(
                out=dst_t[g * 4:(g + 1) * 4], in_=src_t[g * 4:(g + 1) * 4]), (), [RR[g]], q="pool",
                after=[r_.last_w for r_ in gate])

    R_wps = Res("wp_s")
    if stage in ("full", "peer"):
        m1 = sb.mark()
        Wp_b = sb.alloc([128, DC, 2048], BF16, "Wp_stage")
        R_Wp = Res("Wp_stage")
        wq_blk = [sb.alloc([128, D], F32, "wq_blk%d" % i) for i in range(2)]
        R_wqb = [Res("wqb%d" % i) for i in range(2)]
        keysT_sb = sb.alloc([128, 2, 128], F32, "keysT_sb")
        R_keys = Res("keysT")
        DMA(lambda e: e.dma_start(out=keysT_sb[:], in_=keysT_d.rearrange("p j n -> j p n")), w=[R_keys])
        for blk in range(16):
            wb = wq_blk[blk % 2]
            DMA(lambda e, wb=wb, blk=blk: e.dma_start(out=wb[:], in_=wqT_d[blk * 128:(blk + 1) * 128, :]),
                w=[R_wqb[blk % 2]])
            for dcp in range(2):
                pb = 2 + dcp
                for q in range(4):
                    dc = dcp * 4 + q
                    T(lambda e, wb=wb, dc=dc, q=q, pb=pb, blk=blk: e.matmul(
                        PS[pb][:, q * 128:(q + 1) * 128], lhsT=wb[:, dc * 128:(dc + 1) * 128],
                        rhs=keysT_sb[:, blk % 2, :], start=True, stop=True, skip_group_check=True),
                        r=[R_wqb[blk % 2], R_keys], w=[PSR[pb]])
                A(lambda e, dcp=dcp, pb=pb, blk=blk: e.activation(
                    out=Wp_b[:, dcp * 4:(dcp + 1) * 4, blk * 128:(blk + 1) * 128],
                    in_=PS[pb][:, :].rearrange("p (q n) -> p q n", q=4), func=AF.Copy),
                    r=[PSR[pb]], w=[R_Wp])
        DMA(lambda e: e.dma_start(out=wp_s[:, :, :], in_=Wp_b[:]), r=[R_Wp], w=[R_wps])
        S.barrier()
        sb.release(m1)


    R_x1 = [Res("x1_%d" % i) for i in range(NB * 16)]
    R_h2T = [Res("h2T_%d" % i) for i in range(NST)]

    def load_cast(dst_b, src_ap, shape, res, stage_buf, stage_res, eng="dve"):
        DMA(lambda e: e.dma_start(out=stage_buf, in_=src_ap), w=[stage_res])
        if eng == "dve":
            V(lambda e: e.tensor_copy(out=dst_b, in_=stage_buf), r=[stage_res], w=[res])
        else:
            G(lambda e: e.tensor_copy(out=dst_b, in_=stage_buf), r=[stage_res], w=[res])

    if stage in ("full", "mixer"):
        mM = sb.mark()
        w_in_b = sb.alloc([128, DC, 1184], BF16, "w_in_b")
        w_q_b = sb.alloc([128, 3, 768], BF16, "w_q_b")
        w_kv_b = sb.alloc([128, 2, 1024], BF16, "w_kv_b")
        pool_w_b = sb.alloc([128, 4, 128], BF16, "pool_w_b")
        gqk_bc = sb.alloc([128, 2, 96], F32, "gqk_bc")
        ropecs = sb.alloc([128, 16, 2, 32], F32, "ropecs")
        R_w = Res("mixw")
        m1 = sb.mark()
        stgw = sb.alloc([128, DC, 1184], F32, "stgw")
        R_sw = Res("stgw")
        load_cast(w_in_b[:], w_in_d.rearrange("(c p) n -> p c n", p=128), None, R_w, stgw[:], R_sw)
        load_cast(w_q_b[:], w_q_up_d.rearrange("(c p) n -> p c n", p=128), None, R_w,
                  stgw[:, 0:3, 0:768], R_sw)
        load_cast(w_kv_b[:], w_kv_up_d.rearrange("(c p) n -> p c n", p=128), None, R_w,
                  stgw[:, 0:2, 0:1024], R_sw)
        load_cast(pool_w_b[:], pool_w_d.rearrange("g c d -> c g d"), None, R_w, stgw[:, 0:4, 0:128], R_sw)
        DMA(lambda e: e.dma_start(out=gqk_bc[:, 0, :], in_=gqkq_d.partition_broadcast(128)), w=[R_w])
        DMA(lambda e: e.dma_start(out=gqk_bc[:, 1, :], in_=gqkk_d.partition_broadcast(128)), w=[R_w])
        DMA(lambda e: e.dma_start(out=ropecs[:], in_=rope_d[:, :, :, :]), w=[R_w])
        S.barrier()
        sb.release(m1)

        for b in range(NB):
            mb = sb.mark()
            qfT = sb.alloc([128, 8, S_], BF16, "qfT")
            kfT = sb.alloc([128, 8, S_ + CTX], BF16, "kfT")
            vaug = sb.alloc([128, KT, 8, 66], BF16, "vaug")
            pT_off = sb.mark()
            pT = sb.alloc([128, 4, S_ + 16], F32, "pT")
            R_qfT = Res("qfT")
            R_kfT = Res("kfT")
            R_vaug = Res("vaug")
            R_pT = Res("pT")
            V(lambda e: e.memset(vaug[:, :, :, 64:66], 1.0), w=[R_vaug])
            V(lambda e: e.memset(pT[:, :, 0:8], 0.0), w=[R_pT])
            V(lambda e: e.memset(pT[:, :, S_ + 8:S_ + 16], 0.0), w=[R_pT])

            m2 = sb.mark()
            xT_t = sb.alloc([128, DC, TN], F32, "xT_t")
            sq_t = sb.alloc([128, DC, TN], F32, "sq_t")
            h1T = sb.alloc([128, DC, TN], BF16, "h1T")
            rstd_bc = sb.alloc([128, TN], F32, "rstd_bc")
            cqT = sb.alloc([128, 3, TN], BF16, "cqT")
            ckvT = sb.alloc([128, 2, TN], BF16, "ckvT")
            sqc = sb.alloc([128, 5, TN], BF16, "sqc")
            stat = sb.alloc([128, 8], F32, "stat")
            q_f = sb.alloc([128, 8, 96], F32, "q_f")
            k_f = sb.alloc([128, 8, 96], F32, "k_f")
            sq96 = sb.alloc([128, 8, 96], F32, "sq96")
            hst = sb.alloc([128, 16], F32, "hst")
            rtmp = sb.alloc([128, 8, 32], F32, "rtmp")
            qk_b = sb.alloc([128, 8, 96], BF16, "qk_b")
            sq96k = sb.alloc([128, 8, 96], F32, "sq96k")
            hstk = sb.alloc([128, 16], F32, "hstk")
            rtmpk = sb.alloc([128, 8, 32], F32, "rtmpk")
            qk_bk = sb.alloc([128, 8, 96], BF16, "qk_bk")
            R_xT = Res("xT_t")
            R_sqd = [Res("sq_t%d" % i) for i in range(DC)]
            R_h1d = [Res("h1T%d" % i) for i in range(DC)]
            R_rs = Res("rstd")
            R_cq = Res("cqT")
            R_ckv = Res("ckvT")
            R_sqc = Res("sqc")
            R_stat = Res("stat")
            R_qf = Res("q_f")
            R_kf = Res("k_f")
            R_s96 = Res("sq96")
            R_hst = Res("hst")
            R_rt = Res("rtmp")
            R_qkb = Res("qk_b")
            R_s96k = Res("sq96k")
            R_hstk = Res("hstk")
            R_rtk = Res("rtmpk")
            R_qkbk = Res("qk_bk")

            tiles = [(0, t0, TN) for t0 in range(0, S_, TN)] + [(1, 0, CTX)]
            for (is_ctx, t0, nt) in tiles:
                col = 2 if is_ctx else b
                src = ctxT_d[b] if is_ctx else xT_d[b][:, :, t0:t0 + nt]
                DMA(lambda e, src=src, nt=nt: e.dma_start(out=xT_t[:, :, 0:nt], in_=src), w=[R_xT])
                A(lambda e, nt=nt: e.activation(out=sq_t[:, :, 0:nt], in_=xT_t[:, :, 0:nt], func=AF.Square),
                  r=[R_xT], w=R_sqd)
                for dc in range(DC):
                    T(lambda e, dc=dc, nt=nt: e.matmul(PS[0][:, 0:nt], lhsT=ones_f[:], rhs=sq_t[:, dc, 0:nt],
                                                       start=(dc == 0), stop=(dc == DC - 1)),
                      r=[R_sqd[dc], R_const], w=[PSR[0]])
                A(lambda e, nt=nt: e.activation(out=rstd_bc[:, 0:nt], in_=PS[0][:, 0:nt], func=AF.Sqrt,
                                                bias=eps_t[:], scale=1.0 / D), r=[PSR[0], R_const], w=[R_rs])
                V(lambda e, nt=nt: e.reciprocal(out=rstd_bc[:, 0:nt], in_=rstd_bc[:, 0:nt]), r=[R_rs], w=[R_rs])
                for dc in range(DC):
                    V(lambda e, dc=dc, nt=nt, col=col: e.scalar_tensor_tensor(
                        out=sq_t[:, dc, 0:nt], in0=xT_t[:, dc, 0:nt], scalar=gs1[:, dc, col:col + 1],
                        in1=rstd_bc[:, 0:nt], op0=ALU.mult, op1=ALU.mult),
                        r=[R_xT, R_rs, R_mod], w=[R_sqd[dc]])
                    A(lambda e, dc=dc, nt=nt, col=col: e.activation(
                        out=h1T[:, dc, 0:nt], in_=sq_t[:, dc, 0:nt], func=AF.Identity,
                        bias=modfm[:, dc, col:col + 1], scale=1.0), r=[R_sqd[dc], R_mod], w=[R_h1d[dc]])
                ocs = [7, 8] if is_ctx else list(range(9))
                for oc in ocs:
                    pb = 1 + (oc % 2)
                    for dc in range(DC):
                        T(lambda e, oc=oc, dc=dc, nt=nt, pb=pb: e.matmul(
                            PS[pb][:, 0:nt], lhsT=w_in_b[:, dc, oc * 128:(oc + 1) * 128], rhs=h1T[:, dc, 0:nt],
                            start=(dc == 0), stop=(dc == DC - 1)), r=[R_w, R_h1d[dc]], w=[PSR[pb]])
                    if oc < 4:
                        A(lambda e, oc=oc, nt=nt, pb=pb, t0=t0: e.activation(
                            out=pT[:, oc, 8 + t0:8 + t0 + nt], in_=PS[pb][:, 0:nt], func=AF.Copy),
                            r=[PSR[pb]], w=[R_pT])
                    elif oc < 7:
                        j = oc - 4
                        A(lambda e, j=j, nt=nt, pb=pb: e.activation(
                            out=cqT[:, j, 0:nt], in_=PS[pb][:, 0:nt], func=AF.Identity, bias=0.0,
                            scale=gq_fm[:, j:j + 1]), r=[PSR[pb], R_mod], w=[R_cq])
                        A(lambda e, j=j, nt=nt, pb=pb: e.activation(out=sqc[:, j, 0:nt], in_=PS[pb][:, 0:nt],
                                                                    func=AF.Square), r=[PSR[pb]], w=[R_sqc])
                    else:
                        j = oc - 7
                        A(lambda e, j=j, nt=nt, pb=pb: e.activation(
                            out=ckvT[:, j, 0:nt], in_=PS[pb][:, 0:nt], func=AF.Identity, bias=0.0,
                            scale=gkv_fm[:, j:j + 1]), r=[PSR[pb], R_mod], w=[R_ckv])
                        A(lambda e, j=j, nt=nt, pb=pb: e.activation(out=sqc[:, 3 + j, 0:nt], in_=PS[pb][:, 0:nt],
                                                                    func=AF.Square), r=[PSR[pb]], w=[R_sqc])
                cast_some(3, [R_h1d[DC - 1]])
                for ts in range(nt // 128):
                    tsl = slice(ts * 128, (ts + 1) * 128)
                    tg = (t0 // 128 + ts) if not is_ctx else (16 + ts)
                    if not is_ctx:
                        for j in range(3):
                            T(lambda e, j=j, tsl=tsl: e.matmul(PS[3][:, 0:1], lhsT=sqc[:, j, tsl], rhs=ones_b[:, 0:1],
                                                                 start=(j == 0), stop=(j == 2)),
                              r=[R_sqc, R_const], w=[PSR[3]])
                    for j in range(2):
                        T(lambda e, j=j, tsl=tsl: e.matmul(PS[3][:, 2:3], lhsT=sqc[:, 3 + j, tsl], rhs=ones_b[:, 0:1],
                                                             start=(j == 0), stop=(j == 1)),
                          r=[R_sqc, R_const], w=[PSR[3]])
                    for dc in range(DC):
                        T(lambda e, dc=dc, tsl=tsl: e.matmul(PS[3][:, 8:40], lhsT=h1T[:, dc, tsl],
                                                              rhs=w_in_b[:, dc, 1152:1184],
                                                              start=(dc == 0), stop=(dc == DC - 1)),
                          r=[R_h1d[dc], R_w], w=[PSR[3]])
                    if not is_ctx:
                        A(lambda e: e.activation(out=stat[:, 0:1], in_=PS[3][:, 0:1], func=AF.Sqrt, bias=eps_t[:],
                                                 scale=1.0 / 384), r=[PSR[3], R_const], w=[R_stat])
                    A(lambda e: e.activation(out=stat[:, 1:2], in_=PS[3][:, 2:3], func=AF.Sqrt, bias=eps_t[:],
                                             scale=1.0 / 256), r=[PSR[3], R_const], w=[R_stat])
                    V(lambda e: e.reciprocal(out=stat[:, 0:2], in_=stat[:, 0:2]), r=[R_stat], w=[R_stat])

                    def head_norm_rope(buf, R_buf, gi, do_rope, tg, scr):
                        s96, Rs96, hs, Rhs, rt, Rrt, qb, Rqb = scr
                        A(lambda e: e.activation(out=s96[:], in_=buf[:], func=AF.Square),
                          r=[R_buf], w=[Rs96])
                        yield
                        V(lambda e: e.tensor_reduce(out=hs[:, 0:8], in_=s96[:], axis=AX.X, op=ALU.add),
                          r=[Rs96], w=[Rhs])
                        yield
                        A(lambda e: e.activation(out=hs[:, 8:16], in_=hs[:, 0:8], func=AF.Sqrt, bias=eps_t[:],
                                                 scale=1.0 / 96), r=[Rhs, R_const], w=[Rhs])
                        yield
                        V(lambda e: e.reciprocal(out=hs[:, 8:16], in_=hs[:, 8:16]), r=[Rhs], w=[Rhs])
                        yield
                        V(lambda e: e.tensor_tensor(out=buf[:], in0=buf[:],
                                                    in1=hs[:, 8:16].unsqueeze(2).to_broadcast([128, 8, 96]),
                                                    op=ALU.mult), r=[R_buf, Rhs], w=[R_buf])
                        yield
                        V(lambda e: e.tensor_tensor(out=buf[:], in0=buf[:],
                                                    in1=gqk_bc[:, gi, :].unsqueeze(1).to_broadcast([128, 8, 96]),
                                                    op=ALU.mult), r=[R_buf, R_w], w=[R_buf])
                        yield
                        if do_rope:
                            Rv = buf[:, :, 64:96].rearrange("p h (a f n) -> p h a f n", a=2, f=2)
                            Tv = rt[:].rearrange("p h (a f n) -> p h a f n", a=2, f=2)
                            Cs = ropecs[:, tg, 0, :]
                            Sn = ropecs[:, tg, 1, :].rearrange("p (a f n) -> p a f n", a=2, f=2)
                            for f in range(2):
                                V(lambda e, f=f: e.tensor_tensor(
                                    out=Tv[:, :, :, f, :], in0=Rv[:, :, :, 1 - f, :],
                                    in1=Sn[:, :, f, :].unsqueeze(1).to_broadcast([128, 8, 2, 8]), op=ALU.mult),
                                    r=[R_buf, R_w], w=[Rrt])
                                yield
                            V(lambda e: e.tensor_tensor(out=buf[:, :, 64:96], in0=buf[:, :, 64:96],
                                                        in1=Cs.unsqueeze(1).to_broadcast([128, 8, 32]), op=ALU.mult),
                              r=[R_buf, R_w], w=[R_buf])
                            yield
                            V(lambda e: e.tensor_tensor(out=buf[:, :, 64:96], in0=buf[:, :, 64:96], in1=rt[:],
                                                        op=ALU.add), r=[R_buf, Rrt], w=[R_buf])
                            yield
                        V(lambda e: e.tensor_copy(out=qb[:], in_=buf[:]), r=[R_buf], w=[Rqb])
                        yield

                    scr_q = (sq96, R_s96, hst, R_hst, rtmp, R_rt, qk_b, R_qkb)
                    scr_k = (sq96k, R_s96k, hstk, R_hstk, rtmpk, R_rtk, qk_bk, R_qkbk)
                    gens = []
                    if not is_ctx:
                        for (pb, n0, nn) in ((4, 0, 512), (5, 512, 256)):
                            for j in range(3):
                                T(lambda e, pb=pb, n0=n0, nn=nn, j=j, tsl=tsl: e.matmul(
                                    PS[pb][:, 0:nn], lhsT=cqT[:, j, tsl], rhs=w_q_b[:, j, n0:n0 + nn],
                                    start=(j == 0), stop=(j == 2)), r=[R_cq, R_w], w=[PSR[pb]])
                    for (pb, n0) in ((1, 0), (2, 512)):
                        for j in range(2):
                            T(lambda e, pb=pb, n0=n0, j=j, tsl=tsl: e.matmul(
                                PS[pb][:, 0:512], lhsT=ckvT[:, j, tsl], rhs=w_kv_b[:, j, n0:n0 + 512],
                                start=(j == 0), stop=(j == 1)), r=[R_ckv, R_w], w=[PSR[pb]])
                    if not is_ctx:
                        qflat = q_f[:].rearrange("p h d -> p (h d)")
                        A(lambda e, qflat=qflat: e.activation(out=qflat[:, 0:512], in_=PS[4][:, 0:512],
                                                              func=AF.Identity, bias=0.0, scale=stat[:, 0:1]),
                          r=[PSR[4], R_stat], w=[R_qf])
                        A(lambda e, qflat=qflat: e.activation(out=qflat[:, 512:768], in_=PS[5][:, 0:256],
                                                              func=AF.Identity, bias=0.0, scale=stat[:, 0:1]),
                          r=[PSR[5], R_stat], w=[R_qf])
                        gens.append(head_norm_rope(q_f, R_qf, 0, True, tg, scr_q))
                    for (pb, h0) in ((1, 0), (2, 4)):
                        kvv = PS[pb][:].rearrange("p (h d) -> p h d", h=4)
                        A(lambda e, kvv=kvv, h0=h0: e.activation(
                            out=k_f[:, h0:h0 + 4, 0:64], in_=kvv[:, :, 0:64], func=AF.Identity, bias=0.0,
                            scale=stat[:, 1:2]), r=[PSR[pb], R_stat], w=[R_kf])
                        A(lambda e, kvv=kvv, h0=h0, tg=tg: e.activation(
                            out=vaug[:, tg, h0:h0 + 4, 0:64], in_=kvv[:, :, 64:128], func=AF.Identity, bias=0.0,
                            scale=stat[:, 1:2]), r=[PSR[pb], R_stat], w=[R_vaug])
                    V(lambda e: e.tensor_copy(out=k_f[:, :, 64:96],
                                              in_=PS[3][:, 8:40].unsqueeze(1).to_broadcast([128, 8, 32])),
                      r=[PSR[3]], w=[R_kf])
                    gens.append(head_norm_rope(k_f, R_kf, 1, not is_ctx, tg, scr_k))
                    while gens:
                        for gnr in list(gens):
                            if next(gnr, "done") == "done":
                                gens.remove(gnr)
                    if not is_ctx:
                        for h in range(8):
                            T(lambda e, h=h: e.transpose(
                                PS[6][:].bitcast(BF16)[0:96, h * 128:(h + 1) * 128], qk_b[:, h, :], ident_b[:]),
                              r=[R_qkb, R_const], w=[PSR[6]])
                        tq = t0 + ts * 128
                        V(lambda e, tq=tq: e.tensor_copy(
                            out=qfT[0:96, :, tq:tq + 128],
                            in_=PS[6][:].bitcast(BF16)[0:96, :].rearrange("p (h t) -> p h t", h=8)),
                            r=[PSR[6]], w=[R_qfT])
                    for h in range(8):
                        T(lambda e, h=h: e.transpose(
                            PS[7][:].bitcast(BF16)[0:96, h * 128:(h + 1) * 128], qk_bk[:, h, :], ident_b[:]),
                          r=[R_qkbk, R_const], w=[PSR[7]])
                    V(lambda e, tg=tg: e.tensor_copy(
                        out=kfT[0:96, :, tg * 128:(tg + 1) * 128],
                        in_=PS[7][:].bitcast(BF16)[0:96, :].rearrange("p (h t) -> p h t", h=8)),
                        r=[PSR[7]], w=[R_kfT])
            S.barrier()
            sb.release(m2)

            poolT = sb.alloc([128, 4, S_], BF16, "poolT")
            R_poolT = Res("poolT")
            m3b = sb.mark()
            pbuf = [sb.alloc([128, S_ + 16], F32, "pbuf%d" % i) for i in range(2)]
            R_pbuf = [Res("pbuf%d" % i) for i in range(2)]
            difT = sb.alloc([128, S_], BF16, "difT")
            R_dif = Res("difT")
            edg = sb.alloc([128, 4, 16], F32, "edg")
            etmp = sb.alloc([128, 16], F32, "etmp")
            R_edg = Res("edg")
            R_etmp = Res("etmp")
            L = S_ + 16
            DMA(lambda e: e.dma_start(out=edg[:].rearrange("p g t -> p (g t)"),
                                      in_=invcnt_d.rearrange("g t -> (g t)").partition_broadcast(128)), w=[R_edg])
            V(lambda e: e.memset(pbuf[0][:, 0:8], 0.0), w=[R_pbuf[0]])
            V(lambda e: e.memset(pbuf[1][:, 0:8], 0.0), w=[R_pbuf[1]])
            for g, wdw in enumerate((2, 4, 8, 16)):
                src_, Rsrc = pT[:, g, :], R_pT
                sh = 1
                i = 0
                while sh < wdw:
                    dst, Rdst = pbuf[i % 2], R_pbuf[i % 2]
                    V(lambda e, src_=src_, dst=dst, sh=sh: e.tensor_tensor(
                        out=dst[:, sh:L], in0=src_[:, sh:L], in1=src_[:, 0:L - sh], op=ALU.add),
                        r=[Rsrc], w=[Rdst])
                    src_, Rsrc = dst[:, :], Rdst
                    sh *= 2
                    i += 1
                off = 8 + wdw // 2 - 1
                V(lambda e, src_=src_, off=off, g=g, wdw=wdw: e.scalar_tensor_tensor(
                    out=difT[:], in0=src_[:, off:off + S_], scalar=1.0 / wdw, in1=pT[:, g, 8:8 + S_],
                    op0=ALU.mult, op1=ALU.subtract), r=[Rsrc, R_pT], w=[R_dif])
                for (c0, e0) in ((0, 0), (S_ - 8, 8)):
                    V(lambda e, src_=src_, off=off, g=g, c0=c0, e0=e0: e.tensor_tensor(
                        out=etmp[:, e0:e0 + 8], in0=src_[:, off + c0:off + c0 + 8], in1=edg[:, g, e0:e0 + 8],
                        op=ALU.mult), r=[Rsrc, R_edg], w=[R_etmp])
                    V(lambda e, g=g, c0=c0, e0=e0: e.tensor_tensor(
                        out=difT[:, c0:c0 + 8], in0=etmp[:, e0:e0 + 8], in1=pT[:, g, 8 + c0:8 + c0 + 8],
                        op=ALU.subtract), r=[R_etmp, R_pT], w=[R_dif])
                for tt in range(4):
                    pb = tt % 2
                    T(lambda e, g=g, tt=tt, pb=pb: e.matmul(
                        PS[pb][:, :], lhsT=pool_w_b[:, g, :], rhs=difT[:, tt * 512:(tt + 1) * 512],
                        start=True, stop=True), r=[R_w, R_dif], w=[PSR[pb]])
                    A(lambda e, g=g, tt=tt, pb=pb: e.activation(
                        out=poolT[:, g, tt * 512:(tt + 1) * 512], in_=PS[pb][:, :], func=AF.Identity, bias=0.0,
                        scale=psc_fm[:, g:g + 1]), r=[PSR[pb], R_mod], w=[R_poolT])
            S.barrier()
            sb.release(m3b)
            after_poolT = sb.mark()

            sb.top = pT_off
            o_tok = sb.alloc([128, 16, 512], BF16, "o_tok")
            R_otok = Res("o_tok")
            pexp = [sb.alloc([128, 512], BF16, "pexp%d" % i) for i in range(3)]
            R_pexp = [Res("pexp%d" % i) for i in range(3)]
            rz = sb.alloc([128, 4], F32, "rz")
            g2bc = sb.alloc([128, D], F32, "g2bc")
            assert sb.top <= pT_off + 33024
            R_rz = Res("rz")
            sc = 1.0 / np.sqrt(96.0)
            items = [(h, qt, kt) for h in range(8) for qt in range(4) for kt in range(KT)]

            def emit_S(n):
                h, qt, kt = items[n]
                pb = n % 3
                T(lambda e, h=h, qt=qt, kt=kt, pb=pb: e.matmul(
                    PS[pb][:, :], lhsT=kfT[0:96, h, kt * 128:(kt + 1) * 128],
                    rhs=qfT[0:96, h, qt * 512:(qt + 1) * 512], start=True, stop=True),
                    r=[R_kfT, R_qfT], w=[PSR[pb]])
                A(lambda e, pb=pb: e.activation(out=pexp[pb][:], in_=PS[pb][:, :], func=AF.Exp, scale=sc),
                  r=[PSR[pb]], w=[R_pexp[pb]])

            def emit_PV(n):
                h, qt, kt = items[n]
                pb = n % 3
                ob = 4 + ((h * 4 + qt) % 2)
                if qt == 0 and kt == 0:
                    cast_some(5, [R_otok])
                for qs in range(4):
                    T(lambda e, h=h, kt=kt, qs=qs, pb=pb, ob=ob: e.matmul(
                        PS[ob][:, qs * 65:(qs + 1) * 65], lhsT=pexp[pb][:, qs * 128:(qs + 1) * 128],
                        rhs=vaug[:, kt, h, 0:65], start=(kt == 0 and qs == 0),
                        stop=(kt == KT - 1 and qs == 3), skip_group_check=True),
                        r=[R_pexp[pb], R_vaug], w=[PSR[ob]])
                if kt == KT - 1:
                    ov = PS[ob][:, 0:260].rearrange("p (q c) -> p q c", c=65)
                    V(lambda e, ov=ov: e.reciprocal(out=rz[:], in_=ov[:, :, 64]), r=[PSR[ob]], w=[R_rz])
                    V(lambda e, ov=ov, h=h, qt=qt: e.tensor_tensor(
                        out=o_tok[:, qt * 4:(qt + 1) * 4, h * 64:(h + 1) * 64], in0=ov[:, :, 0:64],
                        in1=rz[:].unsqueeze(2).to_broadcast([128, 4, 64]), op=ALU.mult),
                        r=[PSR[ob], R_rz], w=[R_otok])

            emit_S(0)
            emit_S(1)
            for n in range(len(items)):
                if n + 2 < len(items):
                    emit_S(n + 2)
                emit_PV(n)
            S.barrier()
            sb.top = mb
            oT = sb.alloc([128, 4, S_], BF16, "oT")
            R_oT = Res("oT")
            for j in range(16):
                pb = j % 2
                for cc in range(4):
                    T(lambda e, j=j, cc=cc, pb=pb: e.transpose(
                        PS[pb][:].bitcast(BF16)[:, cc * 128:(cc + 1) * 128], o_tok[:, j, cc * 128:(cc + 1) * 128],
                        ident_b[:]), r=[R_otok, R_const], w=[PSR[pb]])
                V(lambda e, j=j, pb=pb: e.tensor_copy(
                    out=oT[:, :, j * 128:(j + 1) * 128],
                    in_=PS[pb][:].bitcast(BF16)[:, 0:512].rearrange("p (c t) -> p c t", c=4)),
                    r=[PSR[pb]], w=[R_oT])
            S.barrier()

            w_out_b = sb.alloc([128, DC, D], BF16, "w_out_b")
            modbc = sb.alloc([128, 3, D], F32, "modbc")
            R_wout = Res("w_out_b")
            R_modbc = Res("modbc")
            m5 = sb.mark()
            stg5 = [sb.alloc([128, DC, 512], F32, "stg5_%d" % i) for i in range(2)]
            R_stg5 = [Res("stg5_%d" % i) for i in range(2)]
            brow = [sb.alloc([1, 512], F32, "brow%d" % i) for i in range(2)]
            R_brow = [Res("brow%d" % i) for i in range(2)]
            silrep = sb.alloc([128, DC, 128], F32, "silrep")
            assert sb.top <= pT_off
            R_m5 = Res("m5misc")
            for hh in range(2):
                DMA(lambda e, hh=hh: e.dma_start(
                    out=stg5[hh][:], in_=w_out_d[:, hh * 512:(hh + 1) * 512].rearrange("(c p) n -> p c n", p=128)),
                    w=[R_stg5[hh]])
                V(lambda e, hh=hh: e.tensor_copy(out=w_out_b[:, :, hh * 512:(hh + 1) * 512], in_=stg5[hh][:]),
                  r=[R_stg5[hh]], w=[R_wout])
            DMA(lambda e: e.dma_start(out=g2bc[:], in_=g2_d.partition_broadcast(128)), w=[R_m5])
            for dc in range(DC):
                V(lambda e, dc=dc, b=b: e.tensor_copy(out=silrep[:, dc, :],
                                                  in_=silT[:, dc, b:b + 1].to_broadcast([128, 128])),
                  r=[R_mod], w=[R_m5])
            for nt8 in range(8):
                sbf = stg5[nt8 % 2]
                c0 = 2048 + nt8 * 512
                DMA(lambda e, sbf=sbf, c0=c0: e.dma_start(
                    out=sbf[:], in_=w_ada_d[:, c0:c0 + 512].rearrange("(c p) n -> p c n", p=128)),
                    w=[R_stg5[nt8 % 2]])
                pb = nt8 % 2
                brw = brow[nt8 % 2]
                DMA(lambda e, brw=brw, c0=c0: e.dma_start(out=brw[:], in_=b_ada_d[c0:c0 + 512].unsqueeze(0)),
                    w=[R_brow[nt8 % 2]])
                for dc in range(DC):
                    T(lambda e, sbf=sbf, dc=dc, pb=pb: e.matmul(PS[pb][:, :], lhsT=silrep[:, dc, :], rhs=sbf[:, dc, :],
                                                                 start=(dc == 0), stop=False),
                      r=[R_m5, R_stg5[nt8 % 2]], w=[PSR[pb]])
                T(lambda e, pb=pb, brw=brw: e.matmul(PS[pb][:, :], lhsT=ones_f[0:1, :],
                                                      rhs=brw[0:1, :], start=False, stop=True),
                  r=[R_brow[nt8 % 2], R_const], w=[PSR[pb]])
                vec, hf = nt8 // 2, nt8 % 2
                if vec == 0:
                    A(lambda e, pb=pb, hf=hf: e.activation(out=modbc[:, 0, hf * 512:(hf + 1) * 512], in_=PS[pb][:, :],
                                                           func=AF.Copy), r=[PSR[pb]], w=[R_modbc])
                elif vec == 1:
                    A(lambda e, pb=pb, hf=hf: e.activation(out=modbc[:, 1, hf * 512:(hf + 1) * 512], in_=PS[pb][:, :],
                                                           func=AF.Copy), r=[PSR[pb]], w=[R_modbc])
                elif vec == 2:
                    V(lambda e, pb=pb, hf=hf: e.scalar_tensor_tensor(
                        out=modbc[:, 2, hf * 512:(hf + 1) * 512], in0=PS[pb][:, :], scalar=1.0,
                        in1=g2bc[:, hf * 512:(hf + 1) * 512], op0=ALU.add, op1=ALU.mult),
                        r=[PSR[pb], R_m5], w=[R_modbc])
                else:
                    A(lambda e, pb=pb, hf=hf, b=b: e.activation(out=gate2_bc[:, b, hf * 512:(hf + 1) * 512],
                                                           in_=PS[pb][:, :], func=AF.Copy),
                      r=[PSR[pb]], w=[R_g2bc[b]])
            S.barrier()
            sb.release(m5)
            x_t = [sb.alloc([128, D], F32, "x_t%d" % i) for i in range(2)]
            R_xt = [Res("x_t%d" % i) for i in range(2)]
            tmp5 = sb.alloc([128, D], F32, "tmp5")
            R_tmp5 = Res("tmp5")
            h2b = [sb.alloc([128, D], BF16, "h2b%d" % i) for i in range(2)]
            R_h2b = [Res("h2b%d" % i) for i in range(2)]
            h2Tt = [sb.alloc([128, DC, 128], BF16, "h2Tt%d" % i) for i in range(2)]
            R_h2Tt = [Res("h2Tt%d" % i) for i in range(2)]
            st5 = sb.alloc([128, 4], F32, "st5")
            R_st5 = Res("st5")

            def m5_front(j):
                i2 = j % 2
                xt = x_t[i2]
                gj = b * 16 + j
                DMA(lambda e, xt=xt, j=j, b=b: e.dma_start(out=xt[:], in_=x_d[b][j * 128:(j + 1) * 128, :]),
                    w=[R_xt[i2]])
                for hf in range(2):
                    pb = 2 * (j % 2) + hf
                    for cc in range(8):
                        lhs = poolT[:, cc, j * 128:(j + 1) * 128] if cc < 4 else oT[:, cc - 4, j * 128:(j + 1) * 128]
                        T(lambda e, lhs=lhs, cc=cc, hf=hf, pb=pb: e.matmul(
                            PS[pb][:, :], lhsT=lhs, rhs=w_out_b[:, cc, hf * 512:(hf + 1) * 512],
                            start=(cc == 0), stop=(cc == 7)), r=[R_poolT, R_oT, R_wout], w=[PSR[pb]])
                    V(lambda e, pb=pb, hf=hf: e.tensor_tensor(
                        out=tmp5[:, hf * 512:(hf + 1) * 512], in0=PS[pb][:, :],
                        in1=modbc[:, 0, hf * 512:(hf + 1) * 512], op=ALU.mult), r=[PSR[pb], R_modbc], w=[R_tmp5])
                V(lambda e, xt=xt: e.tensor_tensor(out=xt[:], in0=xt[:], in1=tmp5[:], op=ALU.add),
                  r=[R_xt[i2], R_tmp5], w=[R_xt[i2]])
                DMA(lambda e, xt=xt, gj=gj: e.dma_start(out=x1_s[gj * 128:(gj + 1) * 128, :], in_=xt[:]),
                    r=[R_xt[i2]], w=[R_x1[gj]])
                A(lambda e, xt=xt: e.activation(out=tmp5[:], in_=xt[:], func=AF.Square, accum_out=st5[:, 0:1]),
                  r=[R_xt[i2]], w=[R_tmp5, R_st5])
                A(lambda e: e.activation(out=st5[:, 1:2], in_=st5[:, 0:1], func=AF.Sqrt, bias=eps_t[:], scale=1.0 / D),
                  r=[R_st5, R_const], w=[R_st5])
                V(lambda e: e.reciprocal(out=st5[:, 2:3], in_=st5[:, 1:2]), r=[R_st5], w=[R_st5])
                V(lambda e, xt=xt: e.scalar_tensor_tensor(out=tmp5[:], in0=xt[:], scalar=st5[:, 2:3],
                                                           in1=modbc[:, 2, :], op0=ALU.mult, op1=ALU.mult),
                  r=[R_xt[i2], R_st5, R_modbc], w=[R_tmp5])
                V(lambda e, i2=i2: e.tensor_tensor(out=h2b[i2][:], in0=tmp5[:], in1=modbc[:, 1, :], op=ALU.add),
                  r=[R_tmp5, R_modbc], w=[R_h2b[i2]])

            def m5_back(j):
                i2 = j % 2
                gj = b * 16 + j
                pbt = 4 + (j % 2)
                for dc in range(DC):
                    T(lambda e, dc=dc, pbt=pbt, i2=i2: e.transpose(
                        PS[pbt][:].bitcast(BF16)[:, dc * 128:(dc + 1) * 128], h2b[i2][:, dc * 128:(dc + 1) * 128],
                        ident_b[:]), r=[R_h2b[i2], R_const], w=[PSR[pbt]])
                ht = h2Tt[i2]
                A(lambda e, ht=ht, pbt=pbt: e.activation(
                    out=ht[:], in_=PS[pbt][:].bitcast(BF16)[:, 0:1024].rearrange("p (c t) -> p c t", c=8),
                    func=AF.Copy), r=[PSR[pbt]], w=[R_h2Tt[i2]])
                stn = gj // 2
                DMA(lambda e, ht=ht, stn=stn, gj=gj: e.dma_start(
                    out=h2T_s[stn][:, :, (gj % 2) * 128:(gj % 2 + 1) * 128], in_=ht[:]),
                    r=[R_h2Tt[i2]], w=[R_h2T[stn]])

            m5_front(0)
            for j in range(16):
                if j + 1 < 16:
                    m5_front(j + 1)
                m5_back(j)
            S.barrier()
            sb.release(mb)
        sb.release(mM)

    if stage in ("full", "peer"):
        cast_some(len(cast_jobs), [])
        Wp_b = sb.alloc([128, DC, 2048], BF16, "Wp_b")
        R_Wp = Res("Wp")
        DMA(lambda e: e.dma_start(out=Wp_b[:], in_=wp_s[:, :, :]), r=[R_wps], w=[R_Wp])

        G_sb = sb.alloc([128, ST, 128], BF16, "G_sb")
        h2T = [sb.alloc([128, DC, ST], BF16, "h2T%d" % i) for i in range(2)]
        x1t = sb.alloc([128, 2, D], F32, "x1t")
        ytmp = sb.alloc([128, 512], F32, "ytmp")
        ubuf = [sb.alloc([128, 4, 1024], BF16, "ubuf%d" % i) for i in range(2)]
        vbuf = [sb.alloc([128, 4, 1024], BF16, "vbuf%d" % i) for i in range(2)]
        OH2 = [sb.alloc([128, 16, 128], BF16, "OH2_%d" % i) for i in range(2)]
        W1 = [sb.alloc([128, 16, 128], BF16, "W1_%d" % i) for i in range(2)]
        vals = sb.alloc([128, 16, 16], F32, "vals")
        idx = sb.alloc([128, 16, 16], U32, "idx")
        tmpl = sb.alloc([128, 256], F32, "tmpl")
        cand = sb.alloc([128, 8, 256], F32, "cand")
        top = sb.alloc([128, 8, 16], F32, "top")
        pos = sb.alloc([128, 8, 16], U32, "pos")
        abi = sb.alloc([128, 2, 128], U32, "abi")
        abf = sb.alloc([128, 2, 128], F32, "abf")
        i12f = sb.alloc([128, 16, 16], F32, "i12f")
        eq = sb.alloc([128, 8, 256], F32, "eq")
        sel = sb.alloc([128, 3, 128], F32, "sel")
        gex = sb.alloc([128, 8, 16], F32, "gex")
        gsum = sb.alloc([128, 16], F32, "gsum")
        selT = [sb.alloc([128, 3, ST], BF16, "selT%d" % i) for i in range(2)]
        gel = [sb.alloc([128, ST], BF16, "gel%d" % i) for i in range(4)]
        GA = [sb.alloc([128, ST], BF16, "GA%d" % i) for i in range(4)]
        R_G = Res("G_sb")
        R_h2Tsb = [Res("h2Tsb%d" % i) for i in range(2)]
        R_x1t = Res("x1t")
        R_ytmp = Res("ytmp")
        R_ubuf = [Res("ubuf%d" % i) for i in range(2)]
        R_vbuf = [Res("vbuf%d" % i) for i in range(2)]
        R_OH2 = [[Res("OH2_%d_%d" % (i, t_)) for t_ in range(16)] for i in range(2)]
        R_W1 = [[Res("W1_%d_%d" % (i, t_)) for t_ in range(16)] for i in range(2)]
        R_tk = Res("topk")
        R_sel = Res("sel")
        R_selT = [Res("selT%d" % i) for i in range(2)]
        R_gel = [Res("gel%d" % i) for i in range(4)]
        R_GA = [Res("GA%d" % i) for i in range(4)]
        R_Ah = [Res("Ahalf%d" % i) for i in range(4)]
        NEG = -1.0e30

        def topk_gen(st):
            hb = st % 2
            DMA(lambda e, st=st, hb=hb: e.dma_start(out=h2T[hb][:], in_=h2T_s[st]), r=[R_h2T[st]], w=[R_h2Tsb[hb]])
            yield
            for tsub in range(2):
                tsl = slice(tsub * 128, (tsub + 1) * 128)
                for nb in range(4):
                    pbk = 7
                    yield
                    yield
                    for dc in range(DC):
                        T(lambda e, nb=nb, dc=dc, tsl=tsl, pbk=pbk, hb=hb: e.matmul(
                            PS[pbk][:, :], lhsT=h2T[hb][:, dc, tsl], rhs=Wp_b[:, dc, nb * 512:(nb + 1) * 512],
                            start=(dc == 0), stop=(dc == DC - 1)), r=[R_h2Tsb[hb], R_Wp], w=[PSR[pbk]])
                    for l4 in range(4):
                        l = nb * 4 + l4
                        c0 = l4 * 128
                        V(lambda e, l=l, pbk=pbk, c0=c0: e.max(out=vals[:, l, 0:8], in_=PS[pbk][:, c0:c0 + 128]),
                          r=[PSR[pbk]], w=[R_tk])
                        V(lambda e, l=l, pbk=pbk, c0=c0: e.match_replace(
                            out=tmpl[:, 0:128], in_to_replace=vals[:, l, 0:8], in_values=PS[pbk][:, c0:c0 + 128],
                            imm_value=NEG), r=[PSR[pbk], R_tk], w=[R_tk])
                        V(lambda e, l=l: e.max(out=vals[:, l, 8:16], in_=tmpl[:, 0:128]), r=[R_tk], w=[R_tk])
                        yield
                        V(lambda e, l=l, pbk=pbk, c0=c0: e.max_index(
                            out=idx[:, l, 0:8], in_max=vals[:, l, 0:8], in_values=PS[pbk][:, c0:c0 + 128]),
                            r=[PSR[pbk], R_tk], w=[R_tk])
                        V(lambda e, l=l, pbk=pbk, c0=c0: e.max_index(
                            out=idx[:, l, 8:16], in_max=vals[:, l, 8:16], in_values=PS[pbk][:, c0:c0 + 128]),
                            r=[PSR[pbk], R_tk], w=[R_tk])
                        yield
                vv = vals[:].rearrange("p (h q) k -> p h q k", q=2)
                V(lambda e, vv=vv: e.tensor_tensor(
                    out=cand[:].rearrange("p h (a b) -> p h a b", a=16),
                    in0=vv[:, :, 0, :].unsqueeze(3).to_broadcast([128, 8, 16, 16]),
                    in1=vv[:, :, 1, :].unsqueeze(2).to_broadcast([128, 8, 16, 16]), op=ALU.add),
                    r=[R_tk], w=[R_tk])
                yield
                for h in range(8):
                    V(lambda e, h=h: e.max(out=top[:, h, 0:8], in_=cand[:, h, :]), r=[R_tk], w=[R_tk])
                    V(lambda e, h=h: e.match_replace(out=tmpl[:, :], in_to_replace=top[:, h, 0:8],
                                                     in_values=cand[:, h, :], imm_value=NEG), r=[R_tk], w=[R_tk])
                    V(lambda e, h=h: e.max(out=top[:, h, 8:16], in_=tmpl[:, :]), r=[R_tk], w=[R_tk])
                    yield
                    V(lambda e, h=h: e.max_index(out=pos[:, h, 0:8], in_max=top[:, h, 0:8], in_values=cand[:, h, :]),
                      r=[R_tk], w=[R_tk])
                    V(lambda e, h=h: e.max_index(out=pos[:, h, 8:16], in_max=top[:, h, 8:16],
                                                 in_values=cand[:, h, :]), r=[R_tk], w=[R_tk])
                    yield
                posf = pos[:].rearrange("p h k -> p (h k)")
                V(lambda e, posf=posf: e.tensor_single_scalar(out=abi[:, 0, :], in_=posf, scalar=4,
                                                              op=ALU.logical_shift_right), r=[R_tk], w=[R_tk])
                V(lambda e, posf=posf: e.tensor_single_scalar(out=abi[:, 1, :], in_=posf, scalar=15,
                                                              op=ALU.bitwise_and), r=[R_tk], w=[R_tk])
                V(lambda e: e.tensor_copy(out=abf[:], in_=abi[:]), r=[R_tk], w=[R_tk])
                V(lambda e: e.tensor_copy(out=i12f[:], in_=idx[:]), r=[R_tk], w=[R_tk])
                yield
                i12v = i12f[:].rearrange("p (h q) k -> p h q k", q=2)
                eqv = eq[:].rearrange("p h (k a) -> p h k a", k=16)
                for which in range(2):
                    af = abf[:, which, :].rearrange("p (h k) -> p h k", h=8)
                    V(lambda e, af=af, eqv=eqv: e.tensor_tensor(
                        out=eqv, in0=af.unsqueeze(3).to_broadcast([128, 8, 16, 16]),
                        in1=iota_f[:, 0:16].unsqueeze(1).unsqueeze(1).to_broadcast([128, 8, 16, 16]),
                        op=ALU.is_equal), r=[R_tk, R_const], w=[R_tk])
                    yield
                    V(lambda e, which=which, eqv=eqv, i12v=i12v: e.tensor_tensor(
                        out=eqv, in0=eqv, in1=i12v[:, :, which, :].unsqueeze(2).to_broadcast([128, 8, 16, 16]),
                        op=ALU.mult), r=[R_tk], w=[R_tk])
                    yield
                    V(lambda e, which=which, eqv=eqv: e.tensor_reduce(
                        out=sel[:, which, :].rearrange("p (h k) -> p h k", h=8), in_=eqv, axis=AX.X, op=ALU.add),
                        r=[R_tk], w=[R_sel])
                    yield
                V(lambda e: e.tensor_tensor(out=gex[:], in0=top[:],
                                            in1=top[:, :, 0:1].to_broadcast([128, 8, 16]), op=ALU.subtract),
                  r=[R_tk], w=[R_tk])
                A(lambda e: e.activation(out=gex[:], in_=gex[:], func=AF.Exp), r=[R_tk], w=[R_tk])
                V(lambda e: e.tensor_reduce(out=gsum[:, 0:8], in_=gex[:], axis=AX.X, op=ALU.add), r=[R_tk], w=[R_tk])
                V(lambda e: e.reciprocal(out=gsum[:, 8:16], in_=gsum[:, 0:8]), r=[R_tk], w=[R_tk])
                V(lambda e: e.tensor_tensor(
                    out=sel[:, 2, :].rearrange("p (h k) -> p h k", h=8), in0=gex[:],
                    in1=gsum[:, 8:16].unsqueeze(2).to_broadcast([128, 8, 16]), op=ALU.mult),
                    r=[R_tk], w=[R_sel])
                yield
                for i in range(3):
                    T(lambda e, i=i: e.transpose(PS[7][:, i * 128:(i + 1) * 128], sel[:, i, :], ident_f[:]),
                      r=[R_sel, R_const], w=[PSR[7]])
                A(lambda e, tsl=tsl, hb=hb: e.activation(
                    out=selT[hb][:, :, tsl], in_=PS[7][:, 0:384].rearrange("p (i t) -> p i t", i=3), func=AF.Copy),
                    r=[PSR[7]], w=[R_selT[hb]])
                yield

        def drain(gen):
            if gen is not None:
                for _ in gen:
                    pass

        def onehot_gen(hb, tgps):
            for tgp in tgps:
                k = tgp % 2
                for tl in range(16):
                    t = tgp * 16 + tl
                    V(lambda e, k=k, tl=tl, t=t, hb=hb: e.tensor_scalar(
                        out=OH2[k][:, tl, :], in0=iota_b[:], scalar1=selT[hb][:, 1, t:t + 1], scalar2=None,
                        op0=ALU.is_equal), r=[R_selT[hb], R_const], w=[R_OH2[k][tl]])
                    V(lambda e, k=k, tl=tl, t=t, hb=hb: e.tensor_scalar(
                        out=W1[k][:, tl, :], in0=iota_b[:], scalar1=selT[hb][:, 0, t:t + 1],
                        scalar2=selT[hb][:, 2, t:t + 1], op0=ALU.is_equal, op1=ALU.mult),
                        r=[R_selT[hb], R_const], w=[R_W1[k][tl]])
                    if tl % 2 == 1:
                        yield

        drain(topk_gen(0))
        prebuilt = 0
        for st in range(NST):
            b = st // (NST // NB)
            hb = st % 2
            for tgp in range(ST // 16):
                k = tgp % 2
                if tgp >= prebuilt:
                    drain(onehot_gen(hb, [tgp]))
                for q4 in range(4):
                    pb = 4 + ((tgp * 4 + q4) % 4)
                    for u in range(4):
                        tl = q4 * 4 + u
                        T(lambda e, k=k, tl=tl, u=u, pb=pb: e.matmul(
                            PS[pb][:, u * 128:(u + 1) * 128], lhsT=OH2[k][:, tl, :], rhs=W1[k][:, tl, :],
                            start=True, stop=True, skip_group_check=True),
                            r=[R_OH2[k][tl], R_W1[k][tl]], w=[PSR[pb]])
                    t0 = tgp * 16 + q4 * 4
                    A(lambda e, pb=pb, t0=t0: e.activation(
                        out=G_sb[:, t0:t0 + 4, :], in_=PS[pb][:, :].rearrange("p (t i) -> p t i", t=4),
                        func=AF.Copy), r=[PSR[pb]], w=[R_G])
            nxt = topk_gen(st + 1) if st + 1 < NST else None
            pre = onehot_gen((st + 1) % 2, [0, 1]) if st + 1 < NST else None
            nxt_done = nxt is None
            DMA(lambda e, st=st: e.dma_start(
                out=x1t[:], in_=x1_s[st * ST:(st + 1) * ST, :].rearrange("(j p) d -> p j d", p=128)),
                r=[R_x1[2 * st], R_x1[2 * st + 1]], w=[R_x1t])
            def emit_A(i1):
                grp, c = i1 // 4, i1 % 4
                kb = grp % 2
                if c == 0:
                    DMA(lambda e, kb=kb, grp=grp: e.dma_start(
                        out=ubuf[kb][:], in_=ub_s[grp * 4:(grp + 1) * 4].rearrange("c p f -> p c f")),
                        r=[R_ub[grp]], w=[R_ubuf[kb]])
                    DMA(lambda e, kb=kb, grp=grp: e.dma_start(
                        out=vbuf[kb][:], in_=vb_s[grp * 4:(grp + 1) * 4].rearrange("c p f -> p c f")),
                        r=[R_vb[grp]], w=[R_vbuf[kb]])
                gi = i1 % 3
                ab = 4 + gi
                for dc in range(DC):
                    T(lambda e, kb=kb, c=c, dc=dc, ab=ab, hb=hb: e.matmul(
                        PS[ab][:, 0:ST], lhsT=ubuf[kb][:, c, dc * 128:(dc + 1) * 128], rhs=h2T[hb][:, dc, :],
                        start=(dc == 0), stop=(dc == DC - 1)),
                        r=[R_ubuf[kb], R_h2Tsb[hb]], w=[PSR[ab]])
                A(lambda e, gi=gi, ab=ab: e.activation(out=gel[gi][:], in_=PS[ab][:, 0:ST], func=AF.Gelu),
                  r=[PSR[ab]], w=[R_gel[gi]])
                G(lambda e, gi=gi, i1=i1: e.tensor_tensor(out=GA[gi][:], in0=gel[gi][:], in1=G_sb[:, :, i1],
                                                          op=ALU.mult), r=[R_gel[gi], R_G], w=[R_GA[gi]])

            def emit_Y(i1):
                grp, c = i1 // 4, i1 % 4
                kb = grp % 2
                gi = i1 % 3
                for ts in range(2):
                    for dh in range(2):
                        T(lambda e, gi=gi, ts=ts, dh=dh, kb=kb, c=c, i1=i1: e.matmul(
                            PS[ts * 2 + dh][:, :], lhsT=GA[gi][:, ts * 128:(ts + 1) * 128],
                            rhs=vbuf[kb][:, c, dh * 512:(dh + 1) * 512], start=(i1 == 0), stop=(i1 == 127)),
                            r=[R_GA[gi], R_vbuf[kb]], w=[PSR[ts * 2 + dh]])

            LA = 2
            for i1 in range(LA):
                emit_A(i1)
            for i1 in range(128):
                if i1 + LA < 128:
                    emit_A(i1 + LA)
                emit_Y(i1)
                if not nxt_done:
                    if next(nxt, "done") == "done":
                        nxt_done = True
                    elif i1 % 2 == 1 and next(nxt, "done") == "done":
                        nxt_done = True
                elif pre is not None:
                    next(pre, None)
            drain(nxt)
            drain(pre)
            prebuilt = 2 if st + 1 < NST else 0
            for ts in range(2):
                for dh in range(2):
                    V(lambda e, ts=ts, dh=dh, b=b: e.tensor_tensor(
                        out=ytmp[:], in0=PS[ts * 2 + dh][:, :], in1=gate2_bc[:, b, dh * 512:(dh + 1) * 512],
                        op=ALU.mult), r=[PSR[ts * 2 + dh], R_g2bc[b]], w=[R_ytmp])
                    V(lambda e, ts=ts, dh=dh: e.tensor_tensor(
                        out=x1t[:, ts, dh * 512:(dh + 1) * 512], in0=x1t[:, ts, dh * 512:(dh + 1) * 512],
                        in1=ytmp[:], op=ALU.add), r=[R_ytmp, R_x1t], w=[R_x1t])
            r0 = (st % (NST // NB)) * ST
            DMA(lambda e, b=b, r0=r0: e.dma_start(
                out=out_d[b][r0:r0 + ST, :].rearrange("(j p) d -> p j d", p=128), in_=x1t[:]),
                r=[R_x1t])

    S.barrier()
    S.emit()
    return nc


def _consts():
    ident = np.eye(128, dtype=np.float32)
    t = np.arange(S_)
    row = (t // 64).astype(np.float32)
    colp = (t % 64).astype(np.float32)
    n = 8
    inv = (1.0 / (np.float32(10000.0) ** (np.arange(n, dtype=np.float32) / np.float32(n)))).astype(np.float32)
    ang_r = row[:, None] * inv
    ang_c = colp[:, None] * inv
    ang = np.concatenate([ang_r, ang_r, ang_c, ang_c], axis=-1).astype(np.float32)
    cos = np.cos(ang).astype(np.float32)
    sin = np.sin(ang).astype(np.float32)
    sgn = np.tile(np.concatenate([-np.ones(8), np.ones(8)]), 2).astype(np.float32)
    sins = sin * sgn[None, :]
    ropecs = np.stack([cos, sins], axis=1)
    ropecs = ropecs.reshape(16, 128, 2, 32).transpose(1, 0, 2, 3).copy()
    invcnt = np.zeros((4, 16), np.float32)
    for g, w in enumerate((2, 4, 8, 16)):
        tt = np.concatenate([np.arange(8), np.arange(S_ - 8, S_)])
        lo = np.clip(tt - w // 2, 0, S_)
        hi = np.clip(tt + w // 2, 0, S_)
        invcnt[g] = 1.0 / (hi - lo).astype(np.float32)
    iota = np.arange(128, dtype=np.float32)
    return dict(ident=ident, ropecs=ropecs, invcnt=invcnt, iota=iota)


def make_in_maps(inp):
    f = lambda a: np.ascontiguousarray(np.asarray(a, dtype=np.float32))
    x = f(inp["x"]); c = f(inp["c"]); ctx = f(inp["ctx"]); c_ctx = f(inp["c_ctx"])
    shared = dict(
        w_ada=f(inp["w_ada"]), b_ada=f(inp["b_ada"]), g_norm1=f(inp["g_norm1"]), w_in=f(inp["w_in"]),
        pool_w=f(inp["pool_w"]), pool_scale=f(inp["pool_scale"]), g_q_lora=f(inp["g_q_lora"]),
        w_q_up=f(inp["w_q_up"]), g_kv_lora=f(inp["g_kv_lora"]), w_kv_up=f(inp["w_kv_up"]),
        g_qk_q=f(inp["g_qk_q"]), g_qk_k=f(inp["g_qk_k"]), w_out=f(inp["w_out"]), g_norm2=f(inp["g_norm2"]),
        wqT=np.ascontiguousarray(f(inp["peer_w_q"]).T),
        keysT=np.ascontiguousarray(f(inp["peer_sub_keys"]).transpose(0, 2, 1)),
        uT=np.ascontiguousarray(f(inp["peer_u"]).reshape(128, 128, 8, 128).transpose(0, 3, 2, 1).reshape(128, 128, 1024)),
        pv=f(inp["peer_v"]).reshape(128, 128, 1024),
    )
    shared.update(_consts())
    maps = []
    for core in range(NCORES):
        bs = slice(core * NB, (core + 1) * NB)
        xb = x[bs]
        m = dict(shared)
        m["x"] = np.ascontiguousarray(xb)
        m["xT"] = np.ascontiguousarray(xb.reshape(NB, S_, DC, 128).transpose(0, 3, 2, 1))
        m["ctxT"] = np.ascontiguousarray(ctx[bs].reshape(NB, CTX, DC, 128).transpose(0, 3, 2, 1))
        cc = np.stack([c[core * NB], c[core * NB + 1], c_ctx], axis=-1)
        m["cT"] = np.ascontiguousarray(cc.reshape(DC, 128, 3).transpose(1, 0, 2))
        maps.append(m)
    return maps


_NC_CACHE = {}


def kernel(**inputs):
    if "full" not in _NC_CACHE:
        _NC_CACHE["full"] = build_program("full")
    nc = _NC_CACHE["full"]
    maps = make_in_maps(inputs)
    res = run_bass_kernel_spmd(nc, maps, core_ids=list(range(NCORES)))
    out = np.concatenate([np.asarray(r["out"]) for r in res.results], axis=0)
    return out.astype(np.float32)
```

```python
import numpy as np
import ml_dtypes
import concourse.bass as bass
import concourse.mybir as mybir
from concourse.bass_utils import run_bass_kernel_spmd

F32 = mybir.dt.float32
BF16 = mybir.dt.bfloat16
U32 = mybir.dt.uint32
I32 = mybir.dt.int32
AF = mybir.ActivationFunctionType
ALU = mybir.AluOpType
AX = mybir.AxisListType

NCORES = 8
NB = 2
S_ = 2048
CTX = 256
D = 1024
DC = 8
KT = (S_ + CTX) // 128
EPS = 1e-6
ST = 256
TN = 256
NST = NB * S_ // ST


class Res:
    __slots__ = ("name", "last_w", "readers")

    def __init__(self, name):
        self.name = name
        self.last_w = None
        self.readers = {}


class Sched:
    ENG = ("pe", "act", "dve", "pool", "sp")

    def __init__(self, nc, n_dma_sems=32):
        self.nc = nc
        self.sem = {e: nc.alloc_semaphore("prog_" + e) for e in self.ENG}
        self.cnt = {e: 0 for e in self.ENG}
        self.dsem = {"sp": [nc.alloc_semaphore("dma_%d" % i) for i in range(n_dma_sems)],
                     "pool": [nc.alloc_semaphore("dmap_%d" % i) for i in range(8)]}
        self.dcnt = {q: [0] * len(v) for q, v in self.dsem.items()}
        self.dnext = {q: 0 for q in self.dsem}
        self.seen = {e: {} for e in self.ENG}
        self.prog = {e: [] for e in self.ENG}

    def _wait(self, eng, ev):
        if ev is None:
            return
        s, v = ev
        k = id(s)
        if self.seen[eng].get(k, 0) >= v:
            return
        self.seen[eng][k] = v
        self.prog[eng].append(("w", s, v))

    def _deps(self, eng, reads, writes, own_sem):
        skip_own = own_sem if eng == "pe" else None
        for r in reads:
            self._wait(eng, r.last_w)
        for w in writes:
            if w.last_w is not None and w.last_w[0] is not skip_own:
                self._wait(eng, w.last_w)
            for ev in w.readers.values():
                if ev[0] is not skip_own:
                    self._wait(eng, ev)

    def _commit(self, ev, reads, writes):
        for w in writes:
            w.last_w = ev
            w.readers = {}
        for r in reads:
            if r not in writes:
                r.readers[id(ev[0])] = ev

    def op(self, eng, fn, reads=(), writes=()):
        own = self.sem[eng]
        self._deps(eng, reads, writes, own)
        self.cnt[eng] += 1
        ev = (own, self.cnt[eng])
        self.prog[eng].append(("o", fn, own, 1))
        self._commit(ev, reads, writes)
        return ev

    def dma(self, fn, reads=(), writes=(), q="sp", after=()):
        for ev in after:
            self._wait(q, ev)
        i = self.dnext[q]
        self.dnext[q] = (i + 1) % len(self.dsem[q])
        s = self.dsem[q][i]
        self._deps(q, reads, writes, None)
        if self.dcnt[q][i] > 0:
            self._wait(q, (s, 16 * self.dcnt[q][i]))
        self.dcnt[q][i] += 1
        ev = (s, 16 * self.dcnt[q][i])
        self.prog[q].append(("o", fn, s, 16))
        self._commit(ev, reads, writes)
        return ev

    def barrier(self):
        evs = [(self.sem[e], self.cnt[e]) for e in self.ENG if self.cnt[e] > 0]
        for q in self.dsem:
            evs += [(self.dsem[q][i], 16 * self.dcnt[q][i]) for i in range(len(self.dsem[q])) if self.dcnt[q][i] > 0]
        for e in self.ENG:
            for ev in evs:
                if ev[0] is not self.sem[e]:
                    self._wait(e, ev)

    def emit(self):
        nc = self.nc
        engmap = {"pe": "tensor", "act": "scalar", "dve": "vector", "pool": "gpsimd", "sp": "sync"}
        with nc.Block() as block:
            for e in self.ENG:
                prog = self.prog[e]

                def body(engine, prog=prog):
                    for it in prog:
                        if it[0] == "w":
                            engine.wait_ge(it[1], it[2])
                        else:
                            it[1](engine).then_inc(it[2], it[3])

                getattr(block, engmap[e])(body)


class SbufStack:
    def __init__(self, nc, lo=16640, hi=229344):
        self.nc = nc
        self.top = lo
        self.hi = hi
        self.n = 0
        self.cache = {}

    def alloc(self, shape, dtype, name=None):
        esz = {F32: 4, BF16: 2, U32: 4, I32: 4}[dtype]
        nbytes = int(np.prod(shape[1:])) * esz
        off = (self.top + 63) // 64 * 64
        assert off + nbytes <= self.hi, ("SBUF overflow", name, off, nbytes)
        self.top = off + nbytes
        self.n += 1
        key = (name, off, tuple(shape), str(dtype))
        if name is not None and key in self.cache:
            return self.cache[key]
        t = self.nc.alloc_sbuf_tensor_at(name or ("t%d" % self.n), list(shape), dtype, offset=off)
        self.cache[key] = t
        return t

    def mark(self):
        return self.top

    def release(self, m):
        self.top = m


def build_program(stage="full"):
    nc = bass.Bass("TRN2", target_bir_lowering=False)

    def din(name, shape, dt=F32):
        return nc.dram_tensor(name, list(shape), dt, kind="ExternalInput").ap()

    dbg = stage != "full"
    xT_d = din("xT", [NB, 128, DC, S_])
    x_d = din("x", [NB, S_, D])
    ctxT_d = din("ctxT", [NB, 128, DC, CTX])
    cT_d = din("cT", [128, DC, 3])
    w_ada_d = din("w_ada", [D, 6 * D])
    b_ada_d = din("b_ada", [6 * D])
    g1_d = din("g_norm1", [D])
    w_in_d = din("w_in", [D, 1184])
    pool_w_d = din("pool_w", [4, 128, 128])
    pool_scale_d = din("pool_scale", [512])
    gq_d = din("g_q_lora", [384])
    w_q_up_d = din("w_q_up", [384, 768])
    gkv_d = din("g_kv_lora", [256])
    w_kv_up_d = din("w_kv_up", [256, 1024])
    gqkq_d = din("g_qk_q", [96])
    gqkk_d = din("g_qk_k", [96])
    w_out_d = din("w_out", [D, D])
    g2_d = din("g_norm2", [D])
    wqT_d = din("wqT", [2048, D])
    keysT_d = din("keysT", [2, 128, 128])
    uT_d = din("uT", [128, 128, 1024])
    v_d = din("pv", [128, 128, 1024])
    ident_d = din("ident", [128, 128])
    rope_d = din("ropecs", [128, 16, 2, 32])
    invcnt_d = din("invcnt", [4, 16])
    iota_d = din("iota", [128])

    out_kind = "ExternalOutput"
    out_d = nc.dram_tensor("out", [NB, S_, D], F32, kind=out_kind).ap()
    skind = "ExternalOutput" if dbg else "Internal"
    x1_s = nc.dram_tensor("x1_s", [NB * S_, D], F32, kind=skind).ap()
    h2T_s = nc.dram_tensor("h2T_s", [NST, 128, DC, ST], BF16, kind=skind).ap()
    ub_s = nc.dram_tensor("ub_s", [128, 128, 1024], BF16, kind="Internal").ap()
    vb_s = nc.dram_tensor("vb_s", [128, 128, 1024], BF16, kind="Internal").ap()
    wp_s = nc.dram_tensor("wp_s", [128, DC, 2048], BF16, kind="Internal").ap()

    S = Sched(nc)
    sb = SbufStack(nc)

    PS = [nc.alloc_psum_tensor("psb%d" % i, [128, 512], F32) for i in range(8)]
    PSR = [Res("psb%d" % i) for i in range(8)]

    def V(fn, r=(), w=()):
        return S.op("dve", fn, r, w)

    def A(fn, r=(), w=()):
        return S.op("act", fn, r, w)

    def G(fn, r=(), w=()):
        return S.op("pool", fn, r, w)

    def T(fn, r=(), w=()):
        return S.op("pe", fn, r, w)

    def DMA(fn, r=(), w=()):
        return S.dma(fn, r, w)

    ident_f = sb.alloc([128, 128], F32, "ident_f")
    ident_b = sb.alloc([128, 128], BF16, "ident_b")
    ones_f = sb.alloc([128, 128], F32, "ones_f")
    ones_b = sb.alloc([128, 128], BF16, "ones_b")
    iota_f = sb.alloc([128, 128], F32, "iota_f")
    iota_b = sb.alloc([128, 128], BF16, "iota_b")
    eps_t = sb.alloc([128, 1], F32, "eps_t")
    R_const = Res("const")
    DMA(lambda e: e.dma_start(out=ident_f[:], in_=ident_d[:, :]), w=[R_const])
    DMA(lambda e: e.dma_start(out=iota_f[:], in_=iota_d.partition_broadcast(128)), w=[R_const])
    V(lambda e: e.tensor_copy(out=ident_b[:], in_=ident_f[:]), r=[R_const], w=[R_const])
    V(lambda e: e.tensor_copy(out=iota_b[:], in_=iota_f[:]), r=[R_const], w=[R_const])
    V(lambda e: e.memset(ones_f[:], 1.0), w=[R_const])
    V(lambda e: e.memset(ones_b[:], 1.0), w=[R_const])
    V(lambda e: e.memset(eps_t[:], EPS), w=[R_const])

    modfm = sb.alloc([128, 16, 3], F32, "modfm")
    gs1 = sb.alloc([128, DC, 3], F32, "gs1")
    silT = sb.alloc([128, DC, 3], F32, "silT")
    gate2_bc = sb.alloc([128, NB, D], F32, "gate2_bc")
    gq_fm = sb.alloc([128, 3], F32, "gq_fm")
    gkv_fm = sb.alloc([128, 2], F32, "gkv_fm")
    psc_fm = sb.alloc([128, 4], F32, "psc_fm")
    R_mod = Res("modfm")
    R_g2bc = [Res("gate2bc%d" % b) for b in range(NB)]
    persist_mark = sb.mark()

    m0 = sb.mark()
    cT = sb.alloc([128, DC, 3], F32, "cT")
    g1_fm = sb.alloc([128, DC], F32, "g1_fm")
    bfm = sb.alloc([128, 16], F32, "bfm")
    wada_c = [sb.alloc([128, DC, 128], F32, "wada_c%d" % i) for i in range(2)]
    R_wc = [Res("wadac%d" % i) for i in range(2)]
    R_t0 = Res("p0tmp")
    DMA(lambda e: e.dma_start(out=cT[:], in_=cT_d[:, :, :]), w=[R_t0])
    DMA(lambda e: e.dma_start(out=g1_fm[:], in_=g1_d.rearrange("(c p) -> p c", p=128), allow_slow_non_contiguous=True), w=[R_t0])
    DMA(lambda e: e.dma_start(out=bfm[:], in_=b_ada_d[0:2048].rearrange("(c p) -> p c", p=128), allow_slow_non_contiguous=True), w=[R_t0])
    DMA(lambda e: e.dma_start(out=gq_fm[:], in_=gq_d.rearrange("(c p) -> p c", p=128), allow_slow_non_contiguous=True), w=[R_mod])
    DMA(lambda e: e.dma_start(out=gkv_fm[:], in_=gkv_d.rearrange("(c p) -> p c", p=128), allow_slow_non_contiguous=True), w=[R_mod])
    DMA(lambda e: e.dma_start(out=psc_fm[:], in_=pool_scale_d.rearrange("(c p) -> p c", p=128), allow_slow_non_contiguous=True), w=[R_mod])
    A(lambda e: e.activation(out=silT[:], in_=cT[:], func=AF.Silu), r=[R_t0], w=[R_mod])
    for j in range(16):
        wb = wada_c[j % 2]
        DMA(lambda e, wb=wb, j=j: e.dma_start(
            out=wb[:], in_=w_ada_d[:, j * 128:(j + 1) * 128].rearrange("(c p) n -> p c n", p=128)),
            w=[R_wc[j % 2]])
        for dc in range(DC):
            T(lambda e, wb=wb, j=j, dc=dc: e.matmul(
                PS[0][:, j * 4:j * 4 + 3], lhsT=wb[:, dc, :], rhs=silT[:, dc, :],
                start=(dc == 0), stop=(dc == DC - 1)),
                r=[R_wc[j % 2], R_mod], w=[PSR[0]])
    V(lambda e: e.tensor_tensor(
        out=modfm[:], in0=PS[0][:, 0:64].rearrange("p (j c) -> p j c", c=4)[:, :, 0:3],
        in1=bfm[:].unsqueeze(2).to_broadcast([128, 16, 3]), op=ALU.add),
        r=[PSR[0], R_t0], w=[R_mod])
    for col in range(3):
        V(lambda e, col=col: e.scalar_tensor_tensor(
            out=gs1[:, :, col], in0=modfm[:, 8:16, col], scalar=1.0, in1=g1_fm[:],
            op0=ALU.add, op1=ALU.mult), r=[R_mod, R_t0], w=[R_mod])
    S.barrier()
    sb.release(m0)

    R_ub = [Res("ub%d" % g) for g in range(32)]
    R_vb = [Res("vb%d" % g) for g in range(32)]
    cast_jobs = []
    if stage in ("full", "peer"):
        for g in range(32):
            for (src_t, dst_t, RR) in ((uT_d, ub_s, R_ub), (v_d, vb_s, R_vb)):
                cast_jobs.append((src_t, dst_t, RR, g))

    def cast_some(n, gate):
        for _ in range(n):
            if not cast_jobs:
                return
            src_t, dst_t, RR, g = cast_jobs.pop(0)
            S.dma(lambda e, src_t=src_t, dst_t=dst_t, g=g: e.dma_start(
                out=dst_t[g * 4:(g + 1) * 4], in_=src_t[g * 4:(g + 1) * 4]), (), [RR[g]], q="pool",
                after=[r_.last_w for r_ in gate])

    R_wps = Res("wp_s")
    if stage in ("full", "peer"):
        m1 = sb.mark()
        Wp_b = sb.alloc([128, DC, 2048], BF16, "Wp_stage")
        R_Wp = Res("Wp_stage")
        wq_blk = [sb.alloc([128, D], F32, "wq_blk%d" % i) for i in range(2)]
        R_wqb = [Res("wqb%d" % i) for i in range(2)]
        keysT_sb = sb.alloc([128, 2, 128], F32, "keysT_sb")
        R_keys = Res("keysT")
        DMA(lambda e: e.dma_start(out=keysT_sb[:], in_=keysT_d.rearrange("p j n -> j p n")), w=[R_keys])
        for blk in range(16):
            wb = wq_blk[blk % 2]
            DMA(lambda e, wb=wb, blk=blk: e.dma_start(out=wb[:], in_=wqT_d[blk * 128:(blk + 1) * 128, :]),
                w=[R_wqb[blk % 2]])
            for dcp in range(2):
                pb = 2 + dcp
                for q in range(4):
                    dc = dcp * 4 + q
                    T(lambda e, wb=wb, dc=dc, q=q, pb=pb, blk=blk: e.matmul(
                        PS[pb][:, q * 128:(q + 1) * 128], lhsT=wb[:, dc * 128:(dc + 1) * 128],
                        rhs=keysT_sb[:, blk % 2, :], start=True, stop=True, skip_group_check=True),
                        r=[R_wqb[blk % 2], R_keys], w=[PSR[pb]])
                A(lambda e, dcp=dcp, pb=pb, blk=blk: e.activation(
                    out=Wp_b[:, dcp * 4:(dcp + 1) * 4, blk * 128:(blk + 1) * 128],
                    in_=PS[pb][:, :].rearrange("p (q n) -> p q n", q=4), func=AF.Copy),
                    r=[PSR[pb]], w=[R_Wp])
        DMA(lambda e: e.dma_start(out=wp_s[:, :, :], in_=Wp_b[:]), r=[R_Wp], w=[R_wps])
        S.barrier()
        sb.release(m1)


    R_x1 = [Res("x1_%d" % i) for i in range(NB * 16)]
    R_h2T = [Res("h2T_%d" % i) for i in range(NST)]

    def load_cast(dst_b, src_ap, shape, res, stage_buf, stage_res, eng="dve"):
        DMA(lambda e: e.dma_start(out=stage_buf, in_=src_ap), w=[stage_res])
        if eng == "dve":
            V(lambda e: e.tensor_copy(out=dst_b, in_=stage_buf), r=[stage_res], w=[res])
        else:
            G(lambda e: e.tensor_copy(out=dst_b, in_=stage_buf), r=[stage_res], w=[res])

    if stage in ("full", "mixer"):
        mM = sb.mark()
        w_in_b = sb.alloc([128, DC, 1184], BF16, "w_in_b")
        w_q_b = sb.alloc([128, 3, 768], BF16, "w_q_b")
        w_kv_b = sb.alloc([128, 2, 1024], BF16, "w_kv_b")
        pool_w_b = sb.alloc([128, 4, 128], BF16, "pool_w_b")
        gqk_bc = sb.alloc([128, 2, 96], F32, "gqk_bc")
        ropecs = sb.alloc([128, 16, 2, 32], F32, "ropecs")
        R_w = Res("mixw")
        m1 = sb.mark()
        stgw = sb.alloc([128, DC, 1184], F32, "stgw")
        R_sw = Res("stgw")
        load_cast(w_in_b[:], w_in_d.rearrange("(c p) n -> p c n", p=128), None, R_w, stgw[:], R_sw)
        load_cast(w_q_b[:], w_q_up_d.rearrange("(c p) n -> p c n", p=128), None, R_w,
                  stgw[:, 0:3, 0:768], R_sw)
        load_cast(w_kv_b[:], w_kv_up_d.rearrange("(c p) n -> p c n", p=128), None, R_w,
                  stgw[:, 0:2, 0:1024], R_sw)
        load_cast(pool_w_b[:], pool_w_d.rearrange("g c d -> c g d"), None, R_w, stgw[:, 0:4, 0:128], R_sw)
        DMA(lambda e: e.dma_start(out=gqk_bc[:, 0, :], in_=gqkq_d.partition_broadcast(128)), w=[R_w])
        DMA(lambda e: e.dma_start(out=gqk_bc[:, 1, :], in_=gqkk_d.partition_broadcast(128)), w=[R_w])
        DMA(lambda e: e.dma_start(out=ropecs[:], in_=rope_d[:, :, :, :]), w=[R_w])
        S.barrier()
        sb.release(m1)

        for b in range(NB):
            mb = sb.mark()
            qfT = sb.alloc([128, 8, S_], BF16, "qfT")
            kfT = sb.alloc([128, 8, S_ + CTX], BF16, "kfT")
            vaug = sb.alloc([128, KT, 8, 66], BF16, "vaug")
            pT_off = sb.mark()
            pT = sb.alloc([128, 4, S_ + 16], F32, "pT")
            R_qfT = Res("qfT")
            R_kfT = Res("kfT")
            R_vaug = Res("vaug")
            R_pT = Res("pT")
            V(lambda e: e.memset(vaug[:, :, :, 64:66], 1.0), w=[R_vaug])
            V(lambda e: e.memset(pT[:, :, 0:8], 0.0), w=[R_pT])
            V(lambda e: e.memset(pT[:, :, S_ + 8:S_ + 16], 0.0), w=[R_pT])

            m2 = sb.mark()
            xT_t = sb.alloc([128, DC, TN], F32, "xT_t")
            sq_t = sb.alloc([128, DC, TN], F32, "sq_t")
            h1T = sb.alloc([128, DC, TN], BF16, "h1T")
            rstd_bc = sb.alloc([128, TN], F32, "rstd_bc")
            cqT = sb.alloc([128, 3, TN], BF16, "cqT")
            ckvT = sb.alloc([128, 2, TN], BF16, "ckvT")
            sqc = sb.alloc([128, 5, TN], BF16, "sqc")
            stat = sb.alloc([128, 8], F32, "stat")
            q_f = sb.alloc([128, 8, 96], F32, "q_f")
            k_f = sb.alloc([128, 8, 96], F32, "k_f")
            sq96 = sb.alloc([128, 8, 96], F32, "sq96")
            hst = sb.alloc([128, 16], F32, "hst")
            rtmp = sb.alloc([128, 8, 32], F32, "rtmp")
            qk_b = sb.alloc([128, 8, 96], BF16, "qk_b")
            sq96k = sb.alloc([128, 8, 96], F32, "sq96k")
            hstk = sb.alloc([128, 16], F32, "hstk")
            rtmpk = sb.alloc([128, 8, 32], F32, "rtmpk")
            qk_bk = sb.alloc([128, 8, 96], BF16, "qk_bk")
            R_xT = Res("xT_t")
            R_sqd = [Res("sq_t%d" % i) for i in range(DC)]
            R_h1d = [Res("h1T%d" % i) for i in range(DC)]
            R_rs = Res("rstd")
            R_cq = Res("cqT")
            R_ckv = Res("ckvT")
            R_sqc = Res("sqc")
            R_stat = Res("stat")
            R_qf = Res("q_f")
            R_kf = Res("k_f")
            R_s96 = Res("sq96")
            R_hst = Res("hst")
            R_rt = Res("rtmp")
            R_qkb = Res("qk_b")
            R_s96k = Res("sq96k")
            R_hstk = Res("hstk")
            R_rtk = Res("rtmpk")
            R_qkbk = Res("qk_bk")

            tiles = [(0, t0, TN) for t0 in range(0, S_, TN)] + [(1, 0, CTX)]
            for (is_ctx, t0, nt) in tiles:
                col = 2 if is_ctx else b
                src = ctxT_d[b] if is_ctx else xT_d[b][:, :, t0:t0 + nt]
                DMA(lambda e, src=src, nt=nt: e.dma_start(out=xT_t[:, :, 0:nt], in_=src), w=[R_xT])
                A(lambda e, nt=nt: e.activation(out=sq_t[:, :, 0:nt], in_=xT_t[:, :, 0:nt], func=AF.Square),
                  r=[R_xT], w=R_sqd)
                for dc in range(DC):
                    T(lambda e, dc=dc, nt=nt: e.matmul(PS[0][:, 0:nt], lhsT=ones_f[:], rhs=sq_t[:, dc, 0:nt],
                                                       start=(dc == 0), stop=(dc == DC - 1)),
                      r=[R_sqd[dc], R_const], w=[PSR[0]])
                A(lambda e, nt=nt: e.activation(out=rstd_bc[:, 0:nt], in_=PS[0][:, 0:nt], func=AF.Sqrt,
                                                bias=eps_t[:], scale=1.0 / D), r=[PSR[0], R_const], w=[R_rs])
                V(lambda e, nt=nt: e.reciprocal(out=rstd_bc[:, 0:nt], in_=rstd_bc[:, 0:nt]), r=[R_rs], w=[R_rs])
                for dc in range(DC):
                    V(lambda e, dc=dc, nt=nt, col=col: e.scalar_tensor_tensor(
                        out=sq_t[:, dc, 0:nt], in0=xT_t[:, dc, 0:nt], scalar=gs1[:, dc, col:col + 1],
                        in1=rstd_bc[:, 0:nt], op0=ALU.mult, op1=ALU.mult),
                        r=[R_xT, R_rs, R_mod], w=[R_sqd[dc]])
                    A(lambda e, dc=dc, nt=nt, col=col: e.activation(
                        out=h1T[:, dc, 0:nt], in_=sq_t[:, dc, 0:nt], func=AF.Identity,
                        bias=modfm[:, dc, col:col + 1], scale=1.0), r=[R_sqd[dc], R_mod], w=[R_h1d[dc]])
                ocs = [7, 8] if is_ctx else list(range(9))
                for oc in ocs:
                    pb = 1 + (oc % 2)
                    for dc in range(DC):
                        T(lambda e, oc=oc, dc=dc, nt=nt, pb=pb: e.matmul(
                            PS[pb][:, 0:nt], lhsT=w_in_b[:, dc, oc * 128:(oc + 1) * 128], rhs=h1T[:, dc, 0:nt],
                            start=(dc == 0), stop=(dc == DC - 1)), r=[R_w, R_h1d[dc]], w=[PSR[pb]])
                    if oc < 4:
                        A(lambda e, oc=oc, nt=nt, pb=pb, t0=t0: e.activation(
                            out=pT[:, oc, 8 + t0:8 + t0 + nt], in_=PS[pb][:, 0:nt], func=AF.Copy),
                            r=[PSR[pb]], w=[R_pT])
                    elif oc < 7:
                        j = oc - 4
                        A(lambda e, j=j, nt=nt, pb=pb: e.activation(
                            out=cqT[:, j, 0:nt], in_=PS[pb][:, 0:nt], func=AF.Identity, bias=0.0,
                            scale=gq_fm[:, j:j + 1]), r=[PSR[pb], R_mod], w=[R_cq])
                        A(lambda e, j=j, nt=nt, pb=pb: e.activation(out=sqc[:, j, 0:nt], in_=PS[pb][:, 0:nt],
                                                                    func=AF.Square), r=[PSR[pb]], w=[R_sqc])
                    else:
                        j = oc - 7
                        A(lambda e, j=j, nt=nt, pb=pb: e.activation(
                            out=ckvT[:, j, 0:nt], in_=PS[pb][:, 0:nt], func=AF.Identity, bias=0.0,
                            scale=gkv_fm[:, j:j + 1]), r=[PSR[pb], R_mod], w=[R_ckv])
                        A(lambda e, j=j, nt=nt, pb=pb: e.activation(out=sqc[:, 3 + j, 0:nt], in_=PS[pb][:, 0:nt],
                                                                    func=AF.Square), r=[PSR[pb]], w=[R_sqc])
                cast_some(3, [R_h1d[DC - 1]])
                for ts in range(nt // 128):
                    tsl = slice(ts * 128, (ts + 1) * 128)
                    tg = (t0 // 128 + ts) if not is_ctx else (16 + ts)
                    if not is_ctx:
                        for j in range(3):
                            T(lambda e, j=j, tsl=tsl: e.matmul(PS[3][:, 0:1], lhsT=sqc[:, j, tsl], rhs=ones_b[:, 0:1],
                                                                 start=(j == 0), stop=(j == 2)),
                              r=[R_sqc, R_const], w=[PSR[3]])
                    for j in range(2):
                        T(lambda e, j=j, tsl=tsl: e.matmul(PS[3][:, 2:3], lhsT=sqc[:, 3 + j, tsl], rhs=ones_b[:, 0:1],
                                                             start=(j == 0), stop=(j == 1)),
                          r=[R_sqc, R_const], w=[PSR[3]])
                    for dc in range(DC):
                        T(lambda e, dc=dc, tsl=tsl: e.matmul(PS[3][:, 8:40], lhsT=h1T[:, dc, tsl],
                                                              rhs=w_in_b[:, dc, 1152:1184],
                                                              start=(dc == 0), stop=(dc == DC - 1)),
                          r=[R_h1d[dc], R_w], w=[PSR[3]])
                    if not is_ctx:
                        A(lambda e: e.activation(out=stat[:, 0:1], in_=PS[3][:, 0:1], func=AF.Sqrt, bias=eps_t[:],
                                                 scale=1.0 / 384), r=[PSR[3], R_const], w=[R_stat])
                    A(lambda e: e.activation(out=stat[:, 1:2], in_=PS[3][:, 2:3], func=AF.Sqrt, bias=eps_t[:],
                                             scale=1.0 / 256), r=[PSR[3], R_const], w=[R_stat])
                    V(lambda e: e.reciprocal(out=stat[:, 0:2], in_=stat[:, 0:2]), r=[R_stat], w=[R_stat])

                    def head_norm_rope(buf, R_buf, gi, do_rope, tg, scr):
                        s96, Rs96, hs, Rhs, rt, Rrt, qb, Rqb = scr
                        A(lambda e: e.activation(out=s96[:], in_=buf[:], func=AF.Square),
                          r=[R_buf], w=[Rs96])
                        yield
                        V(lambda e: e.tensor_reduce(out=hs[:, 0:8], in_=s96[:], axis=AX.X, op=ALU.add),
                          r=[Rs96], w=[Rhs])
                        yield
                        A(lambda e: e.activation(out=hs[:, 8:16], in_=hs[:, 0:8], func=AF.Sqrt, bias=eps_t[:],
                                                 scale=1.0 / 96), r=[Rhs, R_const], w=[Rhs])
                        yield
                        V(lambda e: e.reciprocal(out=hs[:, 8:16], in_=hs[:, 8:16]), r=[Rhs], w=[Rhs])
                        yield
                        V(lambda e: e.tensor_tensor(out=buf[:], in0=buf[:],
                                                    in1=hs[:, 8:16].unsqueeze(2).to_broadcast([128, 8, 96]),
                                                    op=ALU.mult), r=[R_buf, Rhs], w=[R_buf])
                        yield
                        V(lambda e: e.tensor_tensor(out=buf[:], in0=buf[:],
                                                    in1=gqk_bc[:, gi, :].unsqueeze(1).to_broadcast([128, 8, 96]),
                                                    op=ALU.mult), r=[R_buf, R_w], w=[R_buf])
                        yield
                        if do_rope:
                            Rv = buf[:, :, 64:96].rearrange("p h (a f n) -> p h a f n", a=2, f=2)
                            Tv = rt[:].rearrange("p h (a f n) -> p h a f n", a=2, f=2)
                            Cs = ropecs[:, tg, 0, :]
                            Sn = ropecs[:, tg, 1, :].rearrange("p (a f n) -> p a f n", a=2, f=2)
                            for f in range(2):
                                V(lambda e, f=f: e.tensor_tensor(
                                    out=Tv[:, :, :, f, :], in0=Rv[:, :, :, 1 - f, :],
                                    in1=Sn[:, :, f, :].unsqueeze(1).to_broadcast([128, 8, 2, 8]), op=ALU.mult),
                                    r=[R_buf, R_w], w=[Rrt])
                                yield
                            V(lambda e: e.tensor_tensor(out=buf[:, :, 64:96], in0=buf[:, :, 64:96],
                                                        in1=Cs.unsqueeze(1).to_broadcast([128, 8, 32]), op=ALU.mult),
                              r=[R_buf, R_w], w=[R_buf])
                            yield
                            V(lambda e: e.tensor_tensor(out=buf[:, :, 64:96], in0=buf[:, :, 64:96], in1=rt[:],
                                                        op=ALU.add), r=[R_buf, Rrt], w=[R_buf])
                            yield
                        V(lambda e: e.tensor_copy(out=qb[:], in_=buf[:]), r=[R_buf], w=[Rqb])
                        yield

                    scr_q = (sq96, R_s96, hst, R_hst, rtmp, R_rt, qk_b, R_qkb)
                    scr_k = (sq96k, R_s96k, hstk, R_hstk, rtmpk, R_rtk, qk_bk, R_qkbk)
                    gens = []
                    if not is_ctx:
                        for (pb, n0, nn) in ((4, 0, 512), (5, 512, 256)):
                            for j in range(3):
                                T(lambda e, pb=pb, n0=n0, nn=nn, j=j, tsl=tsl: e.matmul(
                                    PS[pb][:, 0:nn], lhsT=cqT[:, j, tsl], rhs=w_q_b[:, j, n0:n0 + nn],
                                    start=(j == 0), stop=(j == 2)), r=[R_cq, R_w], w=[PSR[pb]])
                    for (pb, n0) in ((1, 0), (2, 512)):
                        for j in range(2):
                            T(lambda e, pb=pb, n0=n0, j=j, tsl=tsl: e.matmul(
                                PS[pb][:, 0:512], lhsT=ckvT[:, j, tsl], rhs=w_kv_b[:, j, n0:n0 + 512],
                                start=(j == 0), stop=(j == 1)), r=[R_ckv, R_w], w=[PSR[pb]])
                    if not is_ctx:
                        qflat = q_f[:].rearrange("p h d -> p (h d)")
                        A(lambda e, qflat=qflat: e.activation(out=qflat[:, 0:512], in_=PS[4][:, 0:512],
                                                              func=AF.Identity, bias=0.0, scale=stat[:, 0:1]),
                          r=[PSR[4], R_stat], w=[R_qf])
                        A(lambda e, qflat=qflat: e.activation(out=qflat[:, 512:768], in_=PS[5][:, 0:256],
                                                              func=AF.Identity, bias=0.0, scale=stat[:, 0:1]),
                          r=[PSR[5], R_stat], w=[R_qf])
                        gens.append(head_norm_rope(q_f, R_qf, 0, True, tg, scr_q))
                    for (pb, h0) in ((1, 0), (2, 4)):
                        kvv = PS[pb][:].rearrange("p (h d) -> p h d", h=4)
                        A(lambda e, kvv=kvv, h0=h0: e.activation(
                            out=k_f[:, h0:h0 + 4, 0:64], in_=kvv[:, :, 0:64], func=AF.Identity, bias=0.0,
                            scale=stat[:, 1:2]), r=[PSR[pb], R_stat], w=[R_kf])
                        A(lambda e, kvv=kvv, h0=h0, tg=tg: e.activation(
                            out=vaug[:, tg, h0:h0 + 4, 0:64], in_=kvv[:, :, 64:128], func=AF.Identity, bias=0.0,
                            scale=stat[:, 1:2]), r=[PSR[pb], R_stat], w=[R_vaug])
                    V(lambda e: e.tensor_copy(out=k_f[:, :, 64:96],
                                              in_=PS[3][:, 8:40].unsqueeze(1).to_broadcast([128, 8, 32])),
                      r=[PSR[3]], w=[R_kf])
                    gens.append(head_norm_rope(k_f, R_kf, 1, not is_ctx, tg, scr_k))
                    while gens:
                        for gnr in list(gens):
                            if next(gnr, "done") == "done":
                                gens.remove(gnr)
                    if not is_ctx:
                        for h in range(8):
                            T(lambda e, h=h: e.transpose(
                                PS[6][:].bitcast(BF16)[0:96, h * 128:(h + 1) * 128], qk_b[:, h, :], ident_b[:]),
                              r=[R_qkb, R_const], w=[PSR[6]])
                        tq = t0 + ts * 128
                        V(lambda e, tq=tq: e.tensor_copy(
                            out=qfT[0:96, :, tq:tq + 128],
                            in_=PS[6][:].bitcast(BF16)[0:96, :].rearrange("p (h t) -> p h t", h=8)),
                            r=[PSR[6]], w=[R_qfT])
                    for h in range(8):
                        T(lambda e, h=h: e.transpose(
                            PS[7][:].bitcast(BF16)[0:96, h * 128:(h + 1) * 128], qk_bk[:, h, :], ident_b[:]),
                          r=[R_qkbk, R_const], w=[PSR[7]])
                    V(lambda e, tg=tg: e.tensor_copy(
                        out=kfT[0:96, :, tg * 128:(tg + 1) * 128],
                        in_=PS[7][:].bitcast(BF16)[0:96, :].rearrange("p (h t) -> p h t", h=8)),
                        r=[PSR[7]], w=[R_kfT])
            S.barrier()
            sb.release(m2)

            poolT = sb.alloc([128, 4, S_], BF16, "poolT")
            R_poolT = Res("poolT")
            m3b = sb.mark()
            pbuf = [sb.alloc([128, S_ + 16], F32, "pbuf%d" % i) for i in range(2)]
            R_pbuf = [Res("pbuf%d" % i) for i in range(2)]
            difT = sb.alloc([128, S_], BF16, "difT")
            R_dif = Res("difT")
            edg = sb.alloc([128, 4, 16], F32, "edg")
            etmp = sb.alloc([128, 16], F32, "etmp")
            R_edg = Res("edg")
            R_etmp = Res("etmp")
            L = S_ + 16
            DMA(lambda e: e.dma_start(out=edg[:].rearrange("p g t -> p (g t)"),
                                      in_=invcnt_d.rearrange("g t -> (g t)").partition_broadcast(128)), w=[R_edg])
            V(lambda e: e.memset(pbuf[0][:, 0:8], 0.0), w=[R_pbuf[0]])
            V(lambda e: e.memset(pbuf[1][:, 0:8], 0.0), w=[R_pbuf[1]])
            for g, wdw in enumerate((2, 4, 8, 16)):
                src_, Rsrc = pT[:, g, :], R_pT
                sh = 1
                i = 0
                while sh < wdw:
                    dst, Rdst = pbuf[i % 2], R_pbuf[i % 2]
                    V(lambda e, src_=src_, dst=dst, sh=sh: e.tensor_tensor(
                        out=dst[:, sh:L], in0=src_[:, sh:L], in1=src_[:, 0:L - sh], op=ALU.add),
                        r=[Rsrc], w=[Rdst])
                    src_, Rsrc = dst[:, :], Rdst
                    sh *= 2
                    i += 1
                off = 8 + wdw // 2 - 1
                V(lambda e, src_=src_, off=off, g=g, wdw=wdw: e.scalar_tensor_tensor(
                    out=difT[:], in0=src_[:, off:off + S_], scalar=1.0 / wdw, in1=pT[:, g, 8:8 + S_],
                    op0=ALU.mult, op1=ALU.subtract), r=[Rsrc, R_pT], w=[R_dif])
                for (c0, e0) in ((0, 0), (S_ - 8, 8)):
                    V(lambda e, src_=src_, off=off, g=g, c0=c0, e0=e0: e.tensor_tensor(
                        out=etmp[:, e0:e0 + 8], in0=src_[:, off + c0:off + c0 + 8], in1=edg[:, g, e0:e0 + 8],
                        op=ALU.mult), r=[Rsrc, R_edg], w=[R_etmp])
                    V(lambda e, g=g, c0=c0, e0=e0: e.tensor_tensor(
                        out=difT[:, c0:c0 + 8], in0=etmp[:, e0:e0 + 8], in1=pT[:, g, 8 + c0:8 + c0 + 8],
                        op=ALU.subtract), r=[R_etmp, R_pT], w=[R_dif])
                for tt in range(4):
                    pb = tt % 2
                    T(lambda e, g=g, tt=tt, pb=pb: e.matmul(
                        PS[pb][:, :], lhsT=pool_w_b[:, g, :], rhs=difT[:, tt * 512:(tt + 1) * 512],
                        start=True, stop=True), r=[R_w, R_dif], w=[PSR[pb]])
                    A(lambda e, g=g, tt=tt, pb=pb: e.activation(
                        out=poolT[:, g, tt * 512:(tt + 1) * 512], in_=PS[pb][:, :], func=AF.Identity, bias=0.0,
                        scale=psc_fm[:, g:g + 1]), r=[PSR[pb], R_mod], w=[R_poolT])
            S.barrier()
            sb.release(m3b)
            after_poolT = sb.mark()

            sb.top = pT_off
            o_tok = sb.alloc([128, 16, 512], BF16, "o_tok")
            R_otok = Res("o_tok")
            pexp = [sb.alloc([128, 512], BF16, "pexp%d" % i) for i in range(3)]
            R_pexp = [Res("pexp%d" % i) for i in range(3)]
            rz = sb.alloc([128, 4], F32, "rz")
            g2bc = sb.alloc([128, D], F32, "g2bc")
            assert sb.top <= pT_off + 33024
            R_rz = Res("rz")
            sc = 1.0 / np.sqrt(96.0)
            items = [(h, qt, kt) for h in range(8) for qt in range(4) for kt in range(KT)]

            def emit_S(n):
                h, qt, kt = items[n]
                pb = n % 3
                T(lambda e, h=h, qt=qt, kt=kt, pb=pb: e.matmul(
                    PS[pb][:, :], lhsT=kfT[0:96, h, kt * 128:(kt + 1) * 128],
                    rhs=qfT[0:96, h, qt * 512:(qt + 1) * 512], start=True, stop=True),
                    r=[R_kfT, R_qfT], w=[PSR[pb]])
                A(lambda e, pb=pb: e.activation(out=pexp[pb][:], in_=PS[pb][:, :], func=AF.Exp, scale=sc),
                  r=[PSR[pb]], w=[R_pexp[pb]])

            def emit_PV(n):
                h, qt, kt = items[n]
                pb = n % 3
                ob = 4 + ((h * 4 + qt) % 2)
                if qt == 0 and kt == 0:
                    cast_some(5, [R_otok])
                for qs in range(4):
                    T(lambda e, h=h, kt=kt, qs=qs, pb=pb, ob=ob: e.matmul(
                        PS[ob][:, qs * 65:(qs + 1) * 65], lhsT=pexp[pb][:, qs * 128:(qs + 1) * 128],
                        rhs=vaug[:, kt, h, 0:65], start=(kt == 0 and qs == 0),
                        stop=(kt == KT - 1 and qs == 3), skip_group_check=True),
                        r=[R_pexp[pb], R_vaug], w=[PSR[ob]])
                if kt == KT - 1:
                    ov = PS[ob][:, 0:260].rearrange("p (q c) -> p q c", c=65)
                    V(lambda e, ov=ov: e.reciprocal(out=rz[:], in_=ov[:, :, 64]), r=[PSR[ob]], w=[R_rz])
                    V(lambda e, ov=ov, h=h, qt=qt: e.tensor_tensor(
                        out=o_tok[:, qt * 4:(qt + 1) * 4, h * 64:(h + 1) * 64], in0=ov[:, :, 0:64],
                        in1=rz[:].unsqueeze(2).to_broadcast([128, 4, 64]), op=ALU.mult),
                        r=[PSR[ob], R_rz], w=[R_otok])

            emit_S(0)
            emit_S(1)
            for n in range(len(items)):
                if n + 2 < len(items):
                    emit_S(n + 2)
                emit_PV(n)
            S.barrier()
            sb.top = mb
            oT = sb.alloc([128, 4, S_], BF16, "oT")
            R_oT = Res("oT")
            for j in range(16):
                pb = j % 2
                for cc in range(4):
                    T(lambda e, j=j, cc=cc, pb=pb: e.transpose(
                        PS[pb][:].bitcast(BF16)[:, cc * 128:(cc + 1) * 128], o_tok[:, j, cc * 128:(cc + 1) * 128],
                        ident_b[:]), r=[R_otok, R_const], w=[PSR[pb]])
                V(lambda e, j=j, pb=pb: e.tensor_copy(
                    out=oT[:, :, j * 128:(j + 1) * 128],
                    in_=PS[pb][:].bitcast(BF16)[:, 0:512].rearrange("p (c t) -> p c t", c=4)),
                    r=[PSR[pb]], w=[R_oT])
            S.barrier()

            w_out_b = sb.alloc([128, DC, D], BF16, "w_out_b")
            modbc = sb.alloc([128, 3, D], F32, "modbc")
            R_wout = Res("w_out_b")
            R_modbc = Res("modbc")
            m5 = sb.mark()
            stg5 = [sb.alloc([128, DC, 512], F32, "stg5_%d" % i) for i in range(2)]
            R_stg5 = [Res("stg5_%d" % i) for i in range(2)]
            brow = [sb.alloc([1, 512], F32, "brow%d" % i) for i in range(2)]
            R_brow = [Res("brow%d" % i) for i in range(2)]
            silrep = sb.alloc([128, DC, 128], F32, "silrep")
            assert sb.top <= pT_off
            R_m5 = Res("m5misc")
            for hh in range(2):
                DMA(lambda e, hh=hh: e.dma_start(
                    out=stg5[hh][:], in_=w_out_d[:, hh * 512:(hh + 1) * 512].rearrange("(c p) n -> p c n", p=128)),
                    w=[R_stg5[hh]])
                V(lambda e, hh=hh: e.tensor_copy(out=w_out_b[:, :, hh * 512:(hh + 1) * 512], in_=stg5[hh][:]),
                  r=[R_stg5[hh]], w=[R_wout])
            DMA(lambda e: e.dma_start(out=g2bc[:], in_=g2_d.partition_broadcast(128)), w=[R_m5])
            for dc in range(DC):
                V(lambda e, dc=dc, b=b: e.tensor_copy(out=silrep[:, dc, :],
                                                  in_=silT[:, dc, b:b + 1].to_broadcast([128, 128])),
                  r=[R_mod], w=[R_m5])
            for nt8 in range(8):
                sbf = stg5[nt8 % 2]
                c0 = 2048 + nt8 * 512
                DMA(lambda e, sbf=sbf, c0=c0: e.dma_start(
                    out=sbf[:], in_=w_ada_d[:, c0:c0 + 512].rearrange("(c p) n -> p c n", p=128)),
                    w=[R_stg5[nt8 % 2]])
                pb = nt8 % 2
                brw = brow[nt8 % 2]
                DMA(lambda e, brw=brw, c0=c0: e.dma_start(out=brw[:], in_=b_ada_d[c0:c0 + 512].unsqueeze(0)),
                    w=[R_brow[nt8 % 2]])
                for dc in range(DC):
                    T(lambda e, sbf=sbf, dc=dc, pb=pb: e.matmul(PS[pb][:, :], lhsT=silrep[:, dc, :], rhs=sbf[:, dc, :],
                                                                 start=(dc == 0), stop=False),
                      r=[R_m5, R_stg5[nt8 % 2]], w=[PSR[pb]])
                T(lambda e, pb=pb, brw=brw: e.matmul(PS[pb][:, :], lhsT=ones_f[0:1, :],
                                                      rhs=brw[0:1, :], start=False, stop=True),
                  r=[R_brow[nt8 % 2], R_const], w=[PSR[pb]])
                vec, hf = nt8 // 2, nt8 % 2
                if vec == 0:
                    A(lambda e, pb=pb, hf=hf: e.activation(out=modbc[:, 0, hf * 512:(hf + 1) * 512], in_=PS[pb][:, :],
                                                           func=AF.Copy), r=[PSR[pb]], w=[R_modbc])
                elif vec == 1:
                    A(lambda e, pb=pb, hf=hf: e.activation(out=modbc[:, 1, hf * 512:(hf + 1) * 512], in_=PS[pb][:, :],
                                                           func=AF.Copy), r=[PSR[pb]], w=[R_modbc])
                elif vec == 2:
                    V(lambda e, pb=pb, hf=hf: e.scalar_tensor_tensor(
                        out=modbc[:, 2, hf * 512:(hf + 1) * 512], in0=PS[pb][:, :], scalar=1.0,
                        in1=g2bc[:, hf * 512:(hf + 1) * 512], op0=ALU.add, op1=ALU.mult),
                        r=[PSR[pb], R_m5], w=[R_modbc])
                else:
                    A(lambda e, pb=pb, hf=hf, b=b: e.activation(out=gate2_bc[:, b, hf * 512:(hf + 1) * 512],
                                                           in_=PS[pb][:, :], func=AF.Copy),
                      r=[PSR[pb]], w=[R_g2bc[b]])
            S.barrier()
            sb.release(m5)
            x_t = [sb.alloc([128, D], F32, "x_t%d" % i) for i in range(2)]
            R_xt = [Res("x_t%d" % i) for i in range(2)]
            tmp5 = sb.alloc([128, D], F32, "tmp5")
            R_tmp5 = Res("tmp5")
            h2b = [sb.alloc([128, D], BF16, "h2b%d" % i) for i in range(2)]
            R_h2b = [Res("h2b%d" % i) for i in range(2)]
            h2Tt = [sb.alloc([128, DC, 128], BF16, "h2Tt%d" % i) for i in range(2)]
            R_h2Tt = [Res("h2Tt%d" % i) for i in range(2)]
            st5 = sb.alloc([128, 4], F32, "st5")
            R_st5 = Res("st5")

            def m5_front(j):
                i2 = j % 2
                xt = x_t[i2]
                gj = b * 16 + j
                DMA(lambda e, xt=xt, j=j, b=b: e.dma_start(out=xt[:], in_=x_d[b][j * 128:(j + 1) * 128, :]),
                    w=[R_xt[i2]])
                for hf in range(2):
                    pb = 2 * (j % 2) + hf
                    for cc in range(8):
                        lhs = poolT[:, cc, j * 128:(j + 1) * 128] if cc < 4 else oT[:, cc - 4, j * 128:(j + 1) * 128]
                        T(lambda e, lhs=lhs, cc=cc, hf=hf, pb=pb: e.matmul(
                            PS[pb][:, :], lhsT=lhs, rhs=w_out_b[:, cc, hf * 512:(hf + 1) * 512],
                            start=(cc == 0), stop=(cc == 7)), r=[R_poolT, R_oT, R_wout], w=[PSR[pb]])
                    V(lambda e, pb=pb, hf=hf: e.tensor_tensor(
                        out=tmp5[:, hf * 512:(hf + 1) * 512], in0=PS[pb][:, :],
                        in1=modbc[:, 0, hf * 512:(hf + 1) * 512], op=ALU.mult), r=[PSR[pb], R_modbc], w=[R_tmp5])
                V(lambda e, xt=xt: e.tensor_tensor(out=xt[:], in0=xt[:], in1=tmp5[:], op=ALU.add),
                  r=[R_xt[i2], R_tmp5], w=[R_xt[i2]])
                DMA(lambda e, xt=xt, gj=gj: e.dma_start(out=x1_s[gj * 128:(gj + 1) * 128, :], in_=xt[:]),
                    r=[R_xt[i2]], w=[R_x1[gj]])
                A(lambda e, xt=xt: e.activation(out=tmp5[:], in_=xt[:], func=AF.Square, accum_out=st5[:, 0:1]),
                  r=[R_xt[i2]], w=[R_tmp5, R_st5])
                A(lambda e: e.activation(out=st5[:, 1:2], in_=st5[:, 0:1], func=AF.Sqrt, bias=eps_t[:], scale=1.0 / D),
                  r=[R_st5, R_const], w=[R_st5])
                V(lambda e: e.reciprocal(out=st5[:, 2:3], in_=st5[:, 1:2]), r=[R_st5], w=[R_st5])
                V(lambda e, xt=xt: e.scalar_tensor_tensor(out=tmp5[:], in0=xt[:], scalar=st5[:, 2:3],
                                                           in1=modbc[:, 2, :], op0=ALU.mult, op1=ALU.mult),
                  r=[R_xt[i2], R_st5, R_modbc], w=[R_tmp5])
                V(lambda e, i2=i2: e.tensor_tensor(out=h2b[i2][:], in0=tmp5[:], in1=modbc[:, 1, :], op=ALU.add),
                  r=[R_tmp5, R_modbc], w=[R_h2b[i2]])

            def m5_back(j):
                i2 = j % 2
                gj = b * 16 + j
                pbt = 4 + (j % 2)
                for dc in range(DC):
                    T(lambda e, dc=dc, pbt=pbt, i2=i2: e.transpose(
                        PS[pbt][:].bitcast(BF16)[:, dc * 128:(dc + 1) * 128], h2b[i2][:, dc * 128:(dc + 1) * 128],
                        ident_b[:]), r=[R_h2b[i2], R_const], w=[PSR[pbt]])
                ht = h2Tt[i2]
                A(lambda e, ht=ht, pbt=pbt: e.activation(
                    out=ht[:], in_=PS[pbt][:].bitcast(BF16)[:, 0:1024].rearrange("p (c t) -> p c t", c=8),
                    func=AF.Copy), r=[PSR[pbt]], w=[R_h2Tt[i2]])
                stn = gj // 2
                DMA(lambda e, ht=ht, stn=stn, gj=gj: e.dma_start(
                    out=h2T_s[stn][:, :, (gj % 2) * 128:(gj % 2 + 1) * 128], in_=ht[:]),
                    r=[R_h2Tt[i2]], w=[R_h2T[stn]])

            m5_front(0)
            for j in range(16):
                if j + 1 < 16:
                    m5_front(j + 1)
                m5_back(j)
            S.barrier()
            sb.release(mb)
        sb.release(mM)

    if stage in ("full", "peer"):
        cast_some(len(cast_jobs), [])
        Wp_b = sb.alloc([128, DC, 2048], BF16, "Wp_b")
        R_Wp = Res("Wp")
        DMA(lambda e: e.dma_start(out=Wp_b[:], in_=wp_s[:, :, :]), r=[R_wps], w=[R_Wp])

        G_sb = sb.alloc([128, ST, 128], BF16, "G_sb")
        h2T = [sb.alloc([128, DC, ST], BF16, "h2T%d" % i) for i in range(2)]
        x1t = sb.alloc([128, 2, D], F32, "x1t")
        ytmp = sb.alloc([128, 512], F32, "ytmp")
        ubuf = [sb.alloc([128, 4, 1024], BF16, "ubuf%d" % i) for i in range(2)]
        vbuf = [sb.alloc([128, 4, 1024], BF16, "vbuf%d" % i) for i in range(2)]
        OH2 = [sb.alloc([128, 16, 128], BF16, "OH2_%d" % i) for i in range(3)]
        W1 = [sb.alloc([128, 16, 128], BF16, "W1_%d" % i) for i in range(3)]
        vals = sb.alloc([128, 16, 16], F32, "vals")
        idx = sb.alloc([128, 16, 16], U32, "idx")
        tmpl = sb.alloc([128, 256], F32, "tmpl")
        cand = sb.alloc([128, 8, 256], F32, "cand")
        top = sb.alloc([128, 8, 16], F32, "top")
        pos = sb.alloc([128, 8, 16], U32, "pos")
        abi = sb.alloc([128, 2, 128], U32, "abi")
        abf = sb.alloc([128, 2, 128], F32, "abf")
        i12f = sb.alloc([128, 16, 16], F32, "i12f")
        sel = sb.alloc([128, 3, 128], F32, "sel")
        gex = sb.alloc([128, 8, 16], F32, "gex")
        gsum = sb.alloc([128, 16], F32, "gsum")
        selT = [sb.alloc([128, 3, ST], BF16, "selT%d" % i) for i in range(2)]
        gel = [sb.alloc([128, ST], BF16, "gel%d" % i) for i in range(4)]
        GA = [sb.alloc([128, ST], BF16, "GA%d" % i) for i in range(4)]
        R_G = Res("G_sb")
        R_h2Tsb = [Res("h2Tsb%d" % i) for i in range(2)]
        R_x1t = Res("x1t")
        R_ytmp = Res("ytmp")
        R_ubuf = [Res("ubuf%d" % i) for i in range(2)]
        R_vbuf = [Res("vbuf%d" % i) for i in range(2)]
        R_OH2 = [[Res("OH2_%d_%d" % (i, t_)) for t_ in range(16)] for i in range(3)]
        R_W1 = [[Res("W1_%d_%d" % (i, t_)) for t_ in range(16)] for i in range(3)]
        R_tk = Res("topk")
        R_sel = Res("sel")
        R_selT = [Res("selT%d" % i) for i in range(2)]
        R_gel = [Res("gel%d" % i) for i in range(4)]
        R_GA = [Res("GA%d" % i) for i in range(4)]
        R_Ah = [Res("Ahalf%d" % i) for i in range(4)]
        NEG = -1.0e30

        def topk_gen(st):
            hb = st % 2
            DMA(lambda e, st=st, hb=hb: e.dma_start(out=h2T[hb][:], in_=h2T_s[st]), r=[R_h2T[st]], w=[R_h2Tsb[hb]])
            yield
            for tsub in range(2):
                tsl = slice(tsub * 128, (tsub + 1) * 128)
                for nb in range(4):
                    pbk = 7
                    yield
                    yield
                    for dc in range(DC):
                        T(lambda e, nb=nb, dc=dc, tsl=tsl, pbk=pbk, hb=hb: e.matmul(
                            PS[pbk][:, :], lhsT=h2T[hb][:, dc, tsl], rhs=Wp_b[:, dc, nb * 512:(nb + 1) * 512],
                            start=(dc == 0), stop=(dc == DC - 1)), r=[R_h2Tsb[hb], R_Wp], w=[PSR[pbk]])
                    for l4 in range(4):
                        l = nb * 4 + l4
                        c0 = l4 * 128
                        V(lambda e, l=l, pbk=pbk, c0=c0: e.max(out=vals[:, l, 0:8], in_=PS[pbk][:, c0:c0 + 128]),
                          r=[PSR[pbk]], w=[R_tk])
                        V(lambda e, l=l, pbk=pbk, c0=c0: e.match_replace(
                            out=tmpl[:, 0:128], in_to_replace=vals[:, l, 0:8], in_values=PS[pbk][:, c0:c0 + 128],
                            imm_value=NEG), r=[PSR[pbk], R_tk], w=[R_tk])
                        V(lambda e, l=l: e.max(out=vals[:, l, 8:16], in_=tmpl[:, 0:128]), r=[R_tk], w=[R_tk])
                        yield
                        V(lambda e, l=l, pbk=pbk, c0=c0: e.max_index(
                            out=idx[:, l, 0:8], in_max=vals[:, l, 0:8], in_values=PS[pbk][:, c0:c0 + 128]),
                            r=[PSR[pbk], R_tk], w=[R_tk])
                        V(lambda e, l=l, pbk=pbk, c0=c0: e.max_index(
                            out=idx[:, l, 8:16], in_max=vals[:, l, 8:16], in_values=PS[pbk][:, c0:c0 + 128]),
                            r=[PSR[pbk], R_tk], w=[R_tk])
                        yield
                vv = vals[:].rearrange("p (h q) k -> p h q k", q=2)
                V(lambda e, vv=vv: e.tensor_tensor(
                    out=cand[:].rearrange("p h (a b) -> p h a b", a=16),
                    in0=vv[:, :, 0, :].unsqueeze(3).to_broadcast([128, 8, 16, 16]),
                    in1=vv[:, :, 1, :].unsqueeze(2).to_broadcast([128, 8, 16, 16]), op=ALU.add),
                    r=[R_tk], w=[R_tk])
                yield
                for h in range(8):
                    V(lambda e, h=h: e.max(out=top[:, h, 0:8], in_=cand[:, h, :]), r=[R_tk], w=[R_tk])
                    V(lambda e, h=h: e.match_replace(out=tmpl[:, :], in_to_replace=top[:, h, 0:8],
                                                     in_values=cand[:, h, :], imm_value=NEG), r=[R_tk], w=[R_tk])
                    V(lambda e, h=h: e.max(out=top[:, h, 8:16], in_=tmpl[:, :]), r=[R_tk], w=[R_tk])
                    yield
                    V(lambda e, h=h: e.max_index(out=pos[:, h, 0:8], in_max=top[:, h, 0:8], in_values=cand[:, h, :]),
                      r=[R_tk], w=[R_tk])
                    V(lambda e, h=h: e.max_index(out=pos[:, h, 8:16], in_max=top[:, h, 8:16],
                                                 in_values=cand[:, h, :]), r=[R_tk], w=[R_tk])
                    yield
                posf = pos[:].rearrange("p h k -> p (h k)")
                V(lambda e, posf=posf: e.tensor_single_scalar(out=abi[:, 0, :], in_=posf, scalar=4,
                                                              op=ALU.logical_shift_right), r=[R_tk], w=[R_tk])
                V(lambda e, posf=posf: e.tensor_single_scalar(out=abi[:, 1, :], in_=posf, scalar=15,
                                                              op=ALU.bitwise_and), r=[R_tk], w=[R_tk])
                V(lambda e: e.tensor_copy(out=abf[:], in_=abi[:]), r=[R_tk], w=[R_tk])
                V(lambda e: e.tensor_copy(out=i12f[:], in_=idx[:]), r=[R_tk], w=[R_tk])
                yield
                i12v = i12f[:].rearrange("p (h q) k -> p h q k", q=2)
                eqv = cand[:].rearrange("p h (k a) -> p h k a", k=16)
                for which in range(2):
                    af = abf[:, which, :].rearrange("p (h k) -> p h k", h=8)
                    V(lambda e, af=af, eqv=eqv: e.tensor_tensor(
                        out=eqv, in0=af.unsqueeze(3).to_broadcast([128, 8, 16, 16]),
                        in1=iota_f[:, 0:16].unsqueeze(1).unsqueeze(1).to_broadcast([128, 8, 16, 16]),
                        op=ALU.is_equal), r=[R_tk, R_const], w=[R_tk])
                    yield
                    V(lambda e, which=which, eqv=eqv, i12v=i12v: e.tensor_tensor(
                        out=eqv, in0=eqv, in1=i12v[:, :, which, :].unsqueeze(2).to_broadcast([128, 8, 16, 16]),
                        op=ALU.mult), r=[R_tk], w=[R_tk])
                    yield
                    V(lambda e, which=which, eqv=eqv: e.tensor_reduce(
                        out=sel[:, which, :].rearrange("p (h k) -> p h k", h=8), in_=eqv, axis=AX.X, op=ALU.add),
                        r=[R_tk], w=[R_sel])
                    yield
                V(lambda e: e.tensor_tensor(out=gex[:], in0=top[:],
                                            in1=top[:, :, 0:1].to_broadcast([128, 8, 16]), op=ALU.subtract),
                  r=[R_tk], w=[R_tk])
                A(lambda e: e.activation(out=gex[:], in_=gex[:], func=AF.Exp), r=[R_tk], w=[R_tk])
                V(lambda e: e.tensor_reduce(out=gsum[:, 0:8], in_=gex[:], axis=AX.X, op=ALU.add), r=[R_tk], w=[R_tk])
                V(lambda e: e.reciprocal(out=gsum[:, 8:16], in_=gsum[:, 0:8]), r=[R_tk], w=[R_tk])
                V(lambda e: e.tensor_tensor(
                    out=sel[:, 2, :].rearrange("p (h k) -> p h k", h=8), in0=gex[:],
                    in1=gsum[:, 8:16].unsqueeze(2).to_broadcast([128, 8, 16]), op=ALU.mult),
                    r=[R_tk], w=[R_sel])
                yield
                for i in range(3):
                    T(lambda e, i=i: e.transpose(PS[7][:, i * 128:(i + 1) * 128], sel[:, i, :], ident_f[:]),
                      r=[R_sel, R_const], w=[PSR[7]])
                A(lambda e, tsl=tsl, hb=hb: e.activation(
                    out=selT[hb][:, :, tsl], in_=PS[7][:, 0:384].rearrange("p (i t) -> p i t", i=3), func=AF.Copy),
                    r=[PSR[7]], w=[R_selT[hb]])
                yield

        def drain(gen):
            if gen is not None:
                for _ in gen:
                    pass

        def onehot_gen(hb, tgps):
            for tgp in tgps:
                k = tgp % 3
                for tl in range(16):
                    t = tgp * 16 + tl
                    V(lambda e, k=k, tl=tl, t=t, hb=hb: e.tensor_scalar(
                        out=OH2[k][:, tl, :], in0=iota_b[:], scalar1=selT[hb][:, 1, t:t + 1], scalar2=None,
                        op0=ALU.is_equal), r=[R_selT[hb], R_const], w=[R_OH2[k][tl]])
                    V(lambda e, k=k, tl=tl, t=t, hb=hb: e.tensor_scalar(
                        out=W1[k][:, tl, :], in0=iota_b[:], scalar1=selT[hb][:, 0, t:t + 1],
                        scalar2=selT[hb][:, 2, t:t + 1], op0=ALU.is_equal, op1=ALU.mult),
                        r=[R_selT[hb], R_const], w=[R_W1[k][tl]])
                    if tl % 4 == 3:
                        yield

        drain(topk_gen(0))
        prebuilt = 0
        for st in range(NST):
            b = st // (NST // NB)
            hb = st % 2
            for tgp in range(ST // 16):
                k = tgp % 3
                if tgp >= prebuilt:
                    drain(onehot_gen(hb, [tgp]))
                for q4 in range(4):
                    pb = 4 + ((tgp * 4 + q4) % 4)
                    for u in range(4):
                        tl = q4 * 4 + u
                        T(lambda e, k=k, tl=tl, u=u, pb=pb: e.matmul(
                            PS[pb][:, u * 128:(u + 1) * 128], lhsT=OH2[k][:, tl, :], rhs=W1[k][:, tl, :],
                            start=True, stop=True, skip_group_check=True),
                            r=[R_OH2[k][tl], R_W1[k][tl]], w=[PSR[pb]])
                    t0 = tgp * 16 + q4 * 4
                    A(lambda e, pb=pb, t0=t0: e.activation(
                        out=G_sb[:, t0:t0 + 4, :], in_=PS[pb][:, :].rearrange("p (t i) -> p t i", t=4),
                        func=AF.Copy), r=[PSR[pb]], w=[R_G])
            nxt = topk_gen(st + 1) if st + 1 < NST else None
            pre = onehot_gen((st + 1) % 2, [0, 1, 2]) if st + 1 < NST else None
            nxt_done = nxt is None
            DMA(lambda e, st=st: e.dma_start(
                out=x1t[:], in_=x1_s[st * ST:(st + 1) * ST, :].rearrange("(j p) d -> p j d", p=128)),
                r=[R_x1[2 * st], R_x1[2 * st + 1]], w=[R_x1t])
            def emit_A(i1):
                grp, c = i1 // 4, i1 % 4
                kb = grp % 2
                if c == 0:
                    DMA(lambda e, kb=kb, grp=grp: e.dma_start(
                        out=ubuf[kb][:], in_=ub_s[grp * 4:(grp + 1) * 4].rearrange("c p f -> p c f")),
                        r=[R_ub[grp]], w=[R_ubuf[kb]])
                    DMA(lambda e, kb=kb, grp=grp: e.dma_start(
                        out=vbuf[kb][:], in_=vb_s[grp * 4:(grp + 1) * 4].rearrange("c p f -> p c f")),
                        r=[R_vb[grp]], w=[R_vbuf[kb]])
                gi = i1 % 3
                ab = 4 + gi
                for dc in range(DC):
                    T(lambda e, kb=kb, c=c, dc=dc, ab=ab, hb=hb: e.matmul(
                        PS[ab][:, 0:ST], lhsT=ubuf[kb][:, c, dc * 128:(dc + 1) * 128], rhs=h2T[hb][:, dc, :],
                        start=(dc == 0), stop=(dc == DC - 1)),
                        r=[R_ubuf[kb], R_h2Tsb[hb]], w=[PSR[ab]])
                A(lambda e, gi=gi, ab=ab: e.activation(out=gel[gi][:], in_=PS[ab][:, 0:ST], func=AF.Gelu),
                  r=[PSR[ab]], w=[R_gel[gi]])
                G(lambda e, gi=gi, i1=i1: e.tensor_tensor(out=GA[gi][:], in0=gel[gi][:], in1=G_sb[:, :, i1],
                                                          op=ALU.mult), r=[R_gel[gi], R_G], w=[R_GA[gi]])

            def emit_Y(i1):
                grp, c = i1 // 4, i1 % 4
                kb = grp % 2
                gi = i1 % 3
                for ts in range(2):
                    for dh in range(2):
                        T(lambda e, gi=gi, ts=ts, dh=dh, kb=kb, c=c, i1=i1: e.matmul(
                            PS[ts * 2 + dh][:, :], lhsT=GA[gi][:, ts * 128:(ts + 1) * 128],
                            rhs=vbuf[kb][:, c, dh * 512:(dh + 1) * 512], start=(i1 == 0), stop=(i1 == 127)),
                            r=[R_GA[gi], R_vbuf[kb]], w=[PSR[ts * 2 + dh]])

            LA = 2
            for i1 in range(LA):
                emit_A(i1)
            for i1 in range(128):
                if i1 + LA < 128:
                    emit_A(i1 + LA)
                emit_Y(i1)
                if not nxt_done:
                    if next(nxt, "done") == "done":
                        nxt_done = True
                    elif i1 % 4 == 3 and next(nxt, "done") == "done":
                        nxt_done = True
                elif pre is not None:
                    next(pre, None)
            drain(nxt)
            drain(pre)
            prebuilt = 3 if st + 1 < NST else 0
            for ts in range(2):
                for dh in range(2):
                    V(lambda e, ts=ts, dh=dh, b=b: e.tensor_tensor(
                        out=ytmp[:], in0=PS[ts * 2 + dh][:, :], in1=gate2_bc[:, b, dh * 512:(dh + 1) * 512],
                        op=ALU.mult), r=[PSR[ts * 2 + dh], R_g2bc[b]], w=[R_ytmp])
                    V(lambda e, ts=ts, dh=dh: e.tensor_tensor(
                        out=x1t[:, ts, dh * 512:(dh + 1) * 512], in0=x1t[:, ts, dh * 512:(dh + 1) * 512],
                        in1=ytmp[:], op=ALU.add), r=[R_ytmp, R_x1t], w=[R_x1t])
            r0 = (st % (NST // NB)) * ST
            DMA(lambda e, b=b, r0=r0: e.dma_start(
                out=out_d[b][r0:r0 + ST, :].rearrange("(j p) d -> p j d", p=128), in_=x1t[:]),
                r=[R_x1t])

    S.barrier()
    S.emit()
    return nc


def _consts():
    ident = np.eye(128, dtype=np.float32)
    t = np.arange(S_)
    row = (t // 64).astype(np.float32)
    colp = (t % 64).astype(np.float32)
    n = 8
    inv = (1.0 / (np.float32(10000.0) ** (np.arange(n, dtype=np.float32) / np.float32(n)))).astype(np.float32)
    ang_r = row[:, None] * inv
    ang_c = colp[:, None] * inv
    ang = np.concatenate([ang_r, ang_r, ang_c, ang_c], axis=-1).astype(np.float32)
    cos = np.cos(ang).astype(np.float32)
    sin = np.sin(ang).astype(np.float32)
    sgn = np.tile(np.concatenate([-np.ones(8), np.ones(8)]), 2).astype(np.float32)
    sins = sin * sgn[None, :]
    ropecs = np.stack([cos, sins], axis=1)
    ropecs = ropecs.reshape(16, 128, 2, 32).transpose(1, 0, 2, 3).copy()
    invcnt = np.zeros((4, 16), np.float32)
    for g, w in enumerate((2, 4, 8, 16)):
        tt = np.concatenate([np.arange(8), np.arange(S_ - 8, S_)])
        lo = np.clip(tt - w // 2, 0, S_)
        hi = np.clip(tt + w // 2, 0, S_)
        invcnt[g] = 1.0 / (hi - lo).astype(np.float32)
    iota = np.arange(128, dtype=np.float32)
    return dict(ident=ident, ropecs=ropecs, invcnt=invcnt, iota=iota)


def make_in_maps(inp):
    f = lambda a: np.ascontiguousarray(np.asarray(a, dtype=np.float32))
    x = f(inp["x"]); c = f(inp["c"]); ctx = f(inp["ctx"]); c_ctx = f(inp["c_ctx"])
    shared = dict(
        w_ada=f(inp["w_ada"]), b_ada=f(inp["b_ada"]), g_norm1=f(inp["g_norm1"]), w_in=f(inp["w_in"]),
        pool_w=f(inp["pool_w"]), pool_scale=f(inp["pool_scale"]), g_q_lora=f(inp["g_q_lora"]),
        w_q_up=f(inp["w_q_up"]), g_kv_lora=f(inp["g_kv_lora"]), w_kv_up=f(inp["w_kv_up"]),
        g_qk_q=f(inp["g_qk_q"]), g_qk_k=f(inp["g_qk_k"]), w_out=f(inp["w_out"]), g_norm2=f(inp["g_norm2"]),
        wqT=np.ascontiguousarray(f(inp["peer_w_q"]).T),
        keysT=np.ascontiguousarray(f(inp["peer_sub_keys"]).transpose(0, 2, 1)),
        uT=np.ascontiguousarray(f(inp["peer_u"]).reshape(128, 128, 8, 128).transpose(0, 3, 2, 1).reshape(128, 128, 1024)),
        pv=f(inp["peer_v"]).reshape(128, 128, 1024),
    )
    shared.update(_consts())
    maps = []
    for core in range(NCORES):
        bs = slice(core * NB, (core + 1) * NB)
        xb = x[bs]
        m = dict(shared)
        m["x"] = np.ascontiguousarray(xb)
        m["xT"] = np.ascontiguousarray(xb.reshape(NB, S_, DC, 128).transpose(0, 3, 2, 1))
        m["ctxT"] = np.ascontiguousarray(ctx[bs].reshape(NB, CTX, DC, 128).transpose(0, 3, 2, 1))
        cc = np.stack([c[core * NB], c[core * NB + 1], c_ctx], axis=-1)
        m["cT"] = np.ascontiguousarray(cc.reshape(DC, 128, 3).transpose(1, 0, 2))
        maps.append(m)
    return maps


_NC_CACHE = {}


def kernel(**inputs):
    if "full" not in _NC_CACHE:
        _NC_CACHE["full"] = build_program("full")
    nc = _NC_CACHE["full"]
    maps = make_in_maps(inputs)
    res = run_bass_kernel_spmd(nc, maps, core_ids=list(range(NCORES)))
    out = np.concatenate([np.asarray(r["out"]) for r in res.results], axis=0)
    return out.astype(np.float32)
```
